# Optimizing a Trainium2 kernel written in Bass

```python
import math
import jax, jax.numpy as jnp
from jax import lax
import numpy as np

D_MODEL = 1024
BATCH = 4
SEQ = 4096
DEPTH = 1

D_MIX = D_MODEL
D_CONV = D_MIX // 2
D_ATTN = D_MIX - D_CONV
CONV_WIDTH = 3
CONV_GROUPS = 8
HEAD_DIM = 64
V_DIM = 2 * HEAD_DIM
N_HEADS = D_ATTN // V_DIM
Q_WIDTH = N_HEADS * 2 * HEAD_DIM
D_IN = 3 * D_CONV + 2 * Q_WIDTH + N_HEADS * V_DIM
Q_BLOCK = 128
N_EXPERT_GROUPS = 4
EXPERTS_PER_GROUP = 8
TOP_K = 2
D_EXPERT = D_MODEL // 2
EPS = 1e-6

kernel_name = "hymba_conv_diffattn_hmoe_layer"


def rms_norm(x, g):
    xf = x.astype(jnp.float32)
    y = xf * lax.rsqrt(jnp.mean(xf * xf, axis=-1, keepdims=True) + EPS)
    return (y * g.astype(jnp.float32)).astype(x.dtype)


def lambda_init(layer_idx):
    return 0.8 - 0.6 * math.exp(-0.3 * layer_idx)


def causal_depthwise_conv(u, w):
    K = w.shape[0]
    S = u.shape[1]
    up = jnp.pad(u, ((0, 0), (K - 1, 0), (0, 0)))
    y = up[:, 0:S, :] * w[0]
    for k in range(1, K):
        y = y + up[:, k:k + S, :] * w[k]
    return y


def short_conv_group(xc, b_gate, c_gate, conv_w, conv_out_g):
    y = b_gate * causal_depthwise_conv(c_gate * xc, conv_w)
    Bsz, S, _ = y.shape
    yg = y.reshape(Bsz, S, CONV_GROUPS, D_CONV // CONV_GROUPS)
    yg = rms_norm(yg, jnp.ones((D_CONV // CONV_GROUPS,), y.dtype))
    return yg.reshape(Bsz, S, D_CONV) * conv_out_g.astype(y.dtype)


def diff_attention_group(q, k, v, q_norm_g, k_norm_g, lq1, lk1, lq2, lk2, subln_g, lam_init):
    Bsz, S, _ = q.shape
    q = rms_norm(q.reshape(Bsz, S, N_HEADS, 2, HEAD_DIM), q_norm_g) * (HEAD_DIM ** -0.5)
    k = rms_norm(k.reshape(Bsz, S, N_HEADS, 2, HEAD_DIM), k_norm_g)
    v = v.reshape(Bsz, S, N_HEADS, V_DIM)
    qT = q.transpose(0, 2, 3, 1, 4)
    kT = k.transpose(0, 2, 3, 1, 4)
    vT = v.transpose(0, 2, 1, 3)
    lam = (jnp.exp(jnp.sum(lq1.astype(jnp.float32) * lk1.astype(jnp.float32)))
           - jnp.exp(jnp.sum(lq2.astype(jnp.float32) * lk2.astype(jnp.float32)))
           + lam_init)
    key_pos = jnp.arange(S)
    n_blocks = S // Q_BLOCK

    def one_block(i):
        start = i * Q_BLOCK
        qb = lax.dynamic_slice_in_dim(qT, start, Q_BLOCK, axis=3)
        s = jnp.einsum('bhcqd,bhckd->bhcqk', qb, kT).astype(jnp.float32)
        q_pos = start + jnp.arange(Q_BLOCK)
        mask = key_pos[None, :] <= q_pos[:, None]
        s = jnp.where(mask, s, -jnp.inf)
        p = jax.nn.softmax(s, axis=-1)
        a = (p[:, :, 0] - lam * p[:, :, 1]).astype(vT.dtype)
        return jnp.einsum('bhqk,bhkv->bqhv', a, vT)

    out = lax.map(one_block, jnp.arange(n_blocks))
    out = out.transpose(1, 0, 2, 3, 4).reshape(Bsz, S, N_HEADS, V_DIM)
    out = rms_norm(out, subln_g) * (1.0 - lam_init)
    return out.reshape(Bsz, S, D_ATTN)


def hierarchical_moe(h, w_router_group, w_router_expert, w_gate, w_up, w_down):
    T = h.shape[0]
    G, E = N_EXPERT_GROUPS, EXPERTS_PER_GROUP
    p_group = jax.nn.softmax(jnp.einsum('td,dg->tg', h, w_router_group).astype(jnp.float32), axis=-1)
    g_idx = jnp.argmax(p_group, axis=-1)
    g_gate = jnp.max(p_group, axis=-1)
    logits_e = jnp.einsum('td,de->te', h, w_router_expert).astype(jnp.float32).reshape(T, G, E)
    logits_sel = jnp.take_along_axis(logits_e, g_idx[:, None, None], axis=1)[:, 0]
    top_vals, top_idx = lax.top_k(logits_sel, TOP_K)
    top_w = jax.nn.softmax(top_vals, axis=-1)
    exp_w = jnp.sum(jax.nn.one_hot(top_idx, E, dtype=jnp.float32) * top_w[..., None], axis=1)
    comb = (jax.nn.one_hot(g_idx, G, dtype=jnp.float32)[:, :, None]
            * exp_w[:, None, :] * g_gate[:, None, None]).astype(h.dtype)
    y = jnp.zeros_like(h)
    for g in range(G):
        hg = jnp.einsum('td,edf->tef', h, w_gate[g])
        hu = jnp.einsum('td,edf->tef', h, w_up[g])
        act = jax.nn.silu(hg) * hu * comb[:, g, :, None]
        y = y + jnp.einsum('tef,efd->td', act, w_down[g])
    return y


def setup_inputs(seed: int = 0) -> dict:
    key = jax.random.key(seed)
    ks = jax.random.split(key, 20)
    f32 = jnp.float32
    G, E, F = N_EXPERT_GROUPS, EXPERTS_PER_GROUP, D_EXPERT

    def nrm(k, shape, scale):
        return jax.random.normal(k, shape, f32) * scale

    return {
        "x": nrm(ks[0], (BATCH, SEQ, D_MODEL), 1.0),
        "attn_norm_g": 1.0 + nrm(ks[1], (DEPTH, D_MODEL), 0.02),
        "w_in": nrm(ks[2], (DEPTH, D_MODEL, D_IN), D_MODEL ** -0.5),
        "conv_w": nrm(ks[3], (DEPTH, CONV_WIDTH, D_CONV), CONV_WIDTH ** -0.5),
        "conv_out_g": 1.0 + nrm(ks[4], (DEPTH, D_CONV), 0.02),
        "q_norm_g": 1.0 + nrm(ks[5], (DEPTH, HEAD_DIM), 0.02),
        "k_norm_g": 1.0 + nrm(ks[6], (DEPTH, HEAD_DIM), 0.02),
        "lambda_q1": nrm(ks[7], (DEPTH, HEAD_DIM), 0.1),
        "lambda_k1": nrm(ks[8], (DEPTH, HEAD_DIM), 0.1),
        "lambda_q2": nrm(ks[9], (DEPTH, HEAD_DIM), 0.1),
        "lambda_k2": nrm(ks[10], (DEPTH, HEAD_DIM), 0.1),
        "attn_subln_g": 1.0 + nrm(ks[11], (DEPTH, V_DIM), 0.02),
        "w_out": nrm(ks[12], (DEPTH, D_MIX, D_MODEL), D_MIX ** -0.5),
        "ffn_norm_g": 1.0 + nrm(ks[13], (DEPTH, D_MODEL), 0.02),
        "w_router_group": nrm(ks[14], (DEPTH, D_MODEL, G), D_MODEL ** -0.5),
        "w_router_expert": nrm(ks[15], (DEPTH, D_MODEL, G * E), D_MODEL ** -0.5),
        "w_exp_gate": nrm(ks[16], (DEPTH, G, E, D_MODEL, F), D_MODEL ** -0.5),
        "w_exp_up": nrm(ks[17], (DEPTH, G, E, D_MODEL, F), D_MODEL ** -0.5),
        "w_exp_down": nrm(ks[18], (DEPTH, G, E, F, D_MODEL), F ** -0.5),
    }


def reference(x, attn_norm_g, w_in, conv_w, conv_out_g, q_norm_g, k_norm_g,
              lambda_q1, lambda_k1, lambda_q2, lambda_k2, attn_subln_g, w_out,
              ffn_norm_g, w_router_group, w_router_expert, w_exp_gate, w_exp_up, w_exp_down):
    Bsz, S, D = x.shape
    split_points = [D_CONV, 2 * D_CONV, 3 * D_CONV,
                    3 * D_CONV + Q_WIDTH, 3 * D_CONV + 2 * Q_WIDTH]
    h = x
    for l in range(DEPTH):
        lam_init = lambda_init(l)
        hn = rms_norm(h, attn_norm_g[l])
        proj = jnp.einsum('bsd,de->bse', hn, w_in[l])
        xc, b_gate, c_gate, q, k, v = jnp.split(proj, split_points, axis=-1)
        y_conv = short_conv_group(xc, b_gate, c_gate, conv_w[l], conv_out_g[l])
        y_attn = diff_attention_group(q, k, v, q_norm_g[l], k_norm_g[l],
                                      lambda_q1[l], lambda_k1[l], lambda_q2[l], lambda_k2[l],
                                      attn_subln_g[l], lam_init)
        mix = jnp.concatenate([y_conv, y_attn], axis=-1)
        h = h + jnp.einsum('bse,ed->bsd', mix, w_out[l])
        hn2 = rms_norm(h, ffn_norm_g[l]).reshape(Bsz * S, D)
        y_ffn = hierarchical_moe(hn2, w_router_group[l], w_router_expert[l],
                                 w_exp_gate[l], w_exp_up[l], w_exp_down[l])
        h = h + y_ffn.reshape(Bsz, S, D)
    return h
```

```python
import os
import numpy as np
from contextlib import ExitStack
import concourse.bass as bass
import concourse.mybir as mybir
from concourse.bass_utils import run_bass_kernel_spmd

F32 = mybir.dt.float32
BF16 = mybir.dt.bfloat16
AF = mybir.ActivationFunctionType
ALU = mybir.AluOpType
AX = mybir.AxisListType

ENGS = ["pe", "act", "dve", "pool", "sp"]
EPS = 1e-6
NEXP = 32


class Prog:
    def __init__(self):
        self.ops = {e: [] for e in ENGS}
        self.cnt = {}
        self.waited = {e: {} for e in ENGS}
        self.lastw = {}
        self.readers = {}

    def _waits(self, eng, reads, writes):
        ev = []
        for k in reads:
            if k in self.lastw:
                ev.append(self.lastw[k])
        for k in writes:
            if k in self.lastw:
                ev.append(self.lastw[k])
            ev += self.readers.get(k, [])
        need = {}
        for sk, v in ev:
            if sk == "E_pe" and eng == "pe":
                continue
            if self.waited[eng].get(sk, 0) >= v:
                continue
            if need.get(sk, 0) < v:
                need[sk] = v
        for sk, v in need.items():
            self.waited[eng][sk] = v
        return list(need.items())

    def _book(self, me, reads, writes):
        for k in reads:
            self.readers.setdefault(k, []).append(me)
        for k in writes:
            self.lastw[k] = me
            self.readers[k] = []

    def op(self, eng, fns, reads=(), writes=()):
        if not isinstance(fns, (list, tuple)):
            fns = [fns]
        waits = self._waits(eng, reads, writes)
        sk = "E_" + eng
        self.cnt[sk] = self.cnt.get(sk, 0) + 1
        me = (sk, self.cnt[sk])
        self.ops[eng].append((waits, list(fns), (sk, 1)))
        self._book(me, reads, writes)

    def dma(self, eng, fn, reads=(), writes=(), sem=None):
        waits = self._waits(eng, reads, writes)
        sk = "D_" + (sem if sem is not None else writes[0])
        self.cnt[sk] = self.cnt.get(sk, 0) + 16
        me = (sk, self.cnt[sk])
        self.ops[eng].append((waits, [fn], (sk, 16)))
        self._book(me, reads, writes)

    def barrier(self):
        for eng in ENGS:
            need = []
            for sk, v in self.cnt.items():
                if sk == "E_" + eng:
                    continue
                if self.waited[eng].get(sk, 0) >= v:
                    continue
                self.waited[eng][sk] = v
                need.append((sk, v))
            if need:
                self.ops[eng].append((need, [], None))

    def final_wait(self, eng, keys):
        waits = self._waits(eng, keys, [])
        self.ops[eng].append((waits, [], None))

    def emit(self, nc, stack):
        sems = {}
        for sk in self.cnt:
            sems[sk] = stack.enter_context(nc.semaphore(sk))
        block = stack.enter_context(nc.Block())
        prog = self

        def mk(name):
            def f(e):
                for waits, fns, inc in prog.ops[name]:
                    for sk, v in waits:
                        e.wait_ge(sems[sk], v)
                    if not fns:
                        continue
                    for fn in fns[:-1]:
                        fn(e)
                    ins = fns[-1](e)
                    ins.then_inc(sems[inc[0]], inc[1])
            return f

        block.tensor(mk("pe"))
        block.scalar(mk("act"))
        block.vector(mk("dve"))
        block.gpsimd(mk("pool"))
        block.sync(mk("sp"))


def build_program(n_exp=NEXP, limit=99, dump_at=0):
    nc = bass.Bass("TRN2", target_bir_lowering=False)

    def din(name, shape):
        return nc.dram_tensor(name, list(shape), F32, kind="ExternalInput").ap()

    xk = din("xk", [4096, 1024])
    xh = din("xh", [32, 1024])
    obias_d = din("obias", [128, 2])
    consts_d = din("consts", [128, 384])
    sel_d = din("sel", [32, 4096])
    g_attn = din("attn_norm_g", [1024])
    w_in = din("w_in", [1024, 3072])
    conv_w = din("conv_w", [3, 512])
    conv_out_g = din("conv_out_g", [512])
    q_norm_g = din("q_norm_g", [64])
    k_norm_g = din("k_norm_g", [64])
    lq1 = din("lambda_q1", [64])
    lk1 = din("lambda_k1", [64])
    lq2 = din("lambda_q2", [64])
    lk2 = din("lambda_k2", [64])
    subln_g = din("attn_subln_g", [128])
    w_out = din("w_out", [1024, 1024])
    g_ffn = din("ffn_norm_g", [1024])
    wrg = din("w_router_group", [1024, 4])
    wre = din("w_router_expert", [1024, 32])
    weg = din("w_exp_gate", [32, 1024, 512])
    weu = din("w_exp_up", [32, 1024, 512])
    wed = din("w_exp_down", [32, 512, 1024])
    out = nc.dram_tensor("out", [2048, 1024], F32, kind="ExternalOutput").ap()
    hscr = nc.dram_tensor("hscr", [2048, 1024], F32).ap()

    P = Prog()
    with ExitStack() as st:
        NCOL = 52800
        arena = st.enter_context(nc.sbuf_tensor("arena", [128, NCOL], F32))
        ps = st.enter_context(nc.psum_tensor("ps", [128, 4096], F32))

        class Bump:
            def __init__(self, lo, hi):
                self.p, self.hi = lo, hi

            def f32(self, n):
                a = arena[:, self.p:self.p + n]
                self.p += n
                assert self.p <= self.hi, (self.p, self.hi)
                return a

            def bf(self, n):
                assert n % 2 == 0
                return self.f32(n // 2).bitcast(BF16)

        def bank(k, n=512, off=0):
            return ps[:, k * 512 + off:k * 512 + off + n]

        def finish_early():
            ov = out.rearrange("(p a) n -> p (a n)", p=128)
            for q in range(8):
                if dump_at + (q + 1) * 2048 > NCOL:
                    break
                P.dma("sp", lambda e, q=q: e.dma_start(out=ov[:, q * 2048:(q + 1) * 2048], in_=arena[:, dump_at + q * 2048:dump_at + (q + 1) * 2048]), writes=["out"])
            P.final_wait("sp", ["out"])
            P.emit(nc, st)

        C = Bump(0, 3500)
        identF = C.f32(128)
        triF = C.f32(128)
        b64F = C.f32(128)
        identB = C.bf(128)
        triB = C.bf(128)
        b64B = C.bf(128)
        selF_stage = None
        selB = C.bf(4096)
        ga2 = C.f32(8)
        gf2 = C.f32(8)
        cw = [C.f32(4) for _ in range(3)]
        cog = C.f32(4)
        gq2 = C.f32(1)
        gk2 = C.f32(1)
        sublnc = C.f32(1)
        wosc = C.f32(8)
        obias = C.f32(2)
        lamb = C.f32(256).rearrange("p (a d) -> p a d", a=4)
        lamt = C.f32(8)
        neglam = C.f32(1)
        stats = C.f32(256)
        rsc = C.f32(256)
        stat_i = [0]

        def newstat():
            i = stat_i[0]
            stat_i[0] += 1
            assert i < 256
            return stats[:, i:i + 1], rsc[:, i:i + 1], "st%d" % i

        def rearr_cp(v):
            return v.rearrange("(c p) -> p c", p=128)

        P.dma("sp", lambda e: e.dma_start(out=arena[:, 0:384], in_=consts_d), writes=["cF"])
        P.dma("sp", lambda e: e.dma_start(out=obias, in_=obias_d), writes=["obias"])
        P.dma("sp", lambda e: e.dma_start(out=ga2, in_=rearr_cp(g_attn), allow_slow_non_contiguous=True), writes=["ga2"])
        P.dma("sp", lambda e: e.dma_start(out=gf2, in_=rearr_cp(g_ffn), allow_slow_non_contiguous=True), writes=["gf2"])
        for k in range(3):
            P.dma("sp", lambda e, k=k: e.dma_start(out=cw[k], in_=rearr_cp(conv_w[k]), allow_slow_non_contiguous=True), writes=["convw%d" % k])
        P.dma("sp", lambda e: e.dma_start(out=cog, in_=rearr_cp(conv_out_g), allow_slow_non_contiguous=True), writes=["cog"])
        col = lambda v: v.rearrange("(p o) -> p o", o=1)
        P.dma("sp", lambda e: e.dma_start(out=gq2[0:64, :], in_=col(q_norm_g), allow_slow_non_contiguous=True), writes=["gq2a"])
        P.dma("sp", lambda e: e.dma_start(out=gq2[64:128, :], in_=col(q_norm_g), allow_slow_non_contiguous=True), writes=["gq2b"])
        P.dma("sp", lambda e: e.dma_start(out=gk2[0:64, :], in_=col(k_norm_g), allow_slow_non_contiguous=True), writes=["gk2a"])
        P.dma("sp", lambda e: e.dma_start(out=gk2[64:128, :], in_=col(k_norm_g), allow_slow_non_contiguous=True), writes=["gk2b"])
        P.dma("sp", lambda e: e.dma_start(out=sublnc, in_=col(subln_g), allow_slow_non_contiguous=True), writes=["sublnc"])
        for a, v in enumerate([lq1, lk1, lq2, lk2]):
            P.dma("sp", lambda e, a=a, v=v: e.dma_start(out=lamb[:, a, :], in_=v.partition_broadcast(128)), writes=["lamb%d" % a])
        P.op("pool", lambda e: e.memset(stats, 0.0), writes=["stats"])
        P.op("pool", lambda e: e.tensor_copy(out=identB, in_=identF), reads=["cF"], writes=["identB"])
        P.op("pool", lambda e: e.tensor_copy(out=triB, in_=triF), reads=["cF"], writes=["triB"])
        P.op("pool", lambda e: e.tensor_copy(out=b64B, in_=b64F), reads=["cF"], writes=["b64B"])
        P.op("dve", lambda e: e.tensor_scalar(out=gq2, in0=gq2, scalar1=0.125, scalar2=None, op0=ALU.mult), reads=["gq2a", "gq2b"], writes=["gq2"])
        P.op("dve", lambda e: e.tensor_copy(out=gk2, in_=gk2), reads=["gk2a", "gk2b"], writes=["gk2"])
        P.op("dve", lambda e: e.tensor_copy(out=wosc[:, 0:4], in_=cog), reads=["cog"], writes=["wosc_a"])
        for c in range(4, 8):
            P.op("dve", lambda e, c=c: e.tensor_scalar(out=wosc[:, c:c + 1], in0=sublnc, scalar1=0.8, scalar2=None, op0=ALU.mult), reads=["sublnc"], writes=["wosc%d" % c])
        P.op("dve", lambda e: e.tensor_tensor(out=lamb[:, 0, :], in0=lamb[:, 0, :], in1=lamb[:, 1, :], op=ALU.mult), reads=["lamb0", "lamb1"], writes=["lp0"])
        P.op("dve", lambda e: e.tensor_tensor(out=lamb[:, 2, :], in0=lamb[:, 2, :], in1=lamb[:, 3, :], op=ALU.mult), reads=["lamb2", "lamb3"], writes=["lp1"])
        P.op("dve", lambda e: e.reduce_sum(out=lamt[:, 0:1], in_=lamb[:, 0, :], axis=AX.X), reads=["lp0"], writes=["ls0"])
        P.op("dve", lambda e: e.reduce_sum(out=lamt[:, 1:2], in_=lamb[:, 2, :], axis=AX.X), reads=["lp1"], writes=["ls1"])
        P.op("act", lambda e: e.activation(out=lamt[:, 2:4], in_=lamt[:, 0:2], func=AF.Exp), reads=["ls0", "ls1"], writes=["le"])
        P.op("dve", lambda e: e.tensor_tensor(out=lamt[:, 4:5], in0=lamt[:, 3:4], in1=lamt[:, 2:3], op=ALU.subtract), reads=["le"], writes=["ld"])
        P.op("dve", lambda e: e.tensor_scalar(out=neglam, in0=lamt[:, 4:5], scalar1=-0.2, scalar2=None, op0=ALU.add), reads=["ld"], writes=["neglam"])

        R_KT = 3500
        R_VA = R_KT + 8192
        R_A = R_VA + 8256
        kT = arena[:, R_KT:R_KT + 8192].bitcast(BF16).rearrange("p (h t) -> p h t", h=4)
        VA = arena[:, R_VA:R_VA + 8256].bitcast(BF16).rearrange("p (s h v) -> p s h v", s=32, h=4)
        VA2 = arena[:, R_VA:R_VA + 8256].bitcast(BF16).rearrange("p (s v) -> p s v", v=129)
        P.op("pool", lambda e: e.memset(VA2[:, :, 128:129], 1.0), writes=["VAones"])

        w_in_v = w_in.rearrange("(c p) n -> p c n", p=128)

        def rmsnorm_A(xs_ap, xskey, npart, xn_ap, xnkey, junk_ap):
            ss, rs, sk = newstat()
            P.op("act", lambda e: e.activation(out=junk_ap[0:npart, :], in_=xs_ap[0:npart, :], func=AF.Square, accum_out=ss[0:npart, :]),
                 reads=[xskey, "stats"], writes=[sk, "junk"])
            P.op("act", lambda e: e.activation(out=rs[0:npart, :], in_=ss[0:npart, :], func=AF.Sqrt, scale=1.0 / 1024, bias=EPS), reads=[sk], writes=[sk + "r"])
            P.op("dve", lambda e: e.reciprocal(out=rs[0:npart, :], in_=rs[0:npart, :]), reads=[sk + "r"], writes=[sk + "r"])
            P.op("dve", lambda e: e.tensor_scalar(out=xn_ap[0:npart, :], in0=xs_ap[0:npart, :], scalar1=rs[0:npart, :], scalar2=None, op0=ALU.mult),
                 reads=[xskey, sk + "r"], writes=[xnkey])

        def rmsnorm_B(npart, xn_ap, xnkey, dsts, pst, pstkey):
            P.op("pe", [lambda e, c=c: e.transpose(out=pst[:, c, 0:npart], in_=xn_ap[0:npart, c * 128:(c + 1) * 128], identity=identB[0:npart, 0:npart]) for c in range(8)],
                 reads=[xnkey, "identB"], writes=[pstkey])
            for (dap, dkey, deng) in dsts:
                P.op("act", lambda e, dap=dap: e.copy(out=dap, in_=pst[:, :, 0:npart]), reads=[pstkey], writes=[dkey])

        def norm_feat(psrc, pskey, gcol, gkey, dst, dkey, sqb, sqkey, pS, pSkey, srt, srtkey, src_is_psum=True):
            P.op("act", lambda e: e.activation(out=sqb, in_=psrc, func=AF.Square), reads=[pskey], writes=[sqkey])
            P.op("pe", lambda e: e.matmul(pS, lhsT=b64B, rhs=sqb, start=True, stop=True), reads=[sqkey, "b64B"], writes=[pSkey])
            P.op("act", lambda e: e.activation(out=srt, in_=pS, func=AF.Sqrt, scale=1.0 / 64, bias=EPS), reads=[pSkey], writes=[srtkey])
            P.op("dve", lambda e: e.reciprocal(out=srt, in_=srt), reads=[srtkey], writes=[srtkey])
            if gcol is not None:
                P.op("dve", lambda e: e.scalar_tensor_tensor(out=dst, in0=psrc, scalar=gcol, in1=srt, op0=ALU.mult, op1=ALU.mult),
                     reads=[pskey, srtkey, gkey], writes=[dkey])
            else:
                P.op("dve", lambda e: e.tensor_tensor(out=dst, in0=psrc, in1=srt, op=ALU.mult), reads=[pskey, srtkey], writes=[dkey])

        if limit == 0:
            finish_early()
            return nc
        B1 = Bump(R_A, NCOL)
        hnTo = B1.bf(8 * 2080).rearrange("p (c t) -> p c t", c=8)
        wk_bf = B1.bf(8 * 512).rearrange("p (c n) -> p c n", c=8)
        wv_bf = B1.bf(8 * 512).rearrange("p (c n) -> p c n", c=8)
        wst = B1.f32(4096).rearrange("p (c n) -> p c n", c=8)
        hng = [B1.bf(8 * 512).rearrange("p (c t) -> p c t", c=8) for _ in range(2)]
        xs = [B1.f32(1024) for _ in range(2)]
        xn = [B1.bf(1024) for _ in range(2)]
        junk = B1.bf(1024)
        sqb = [B1.bf(512) for _ in range(2)]
        srt = [B1.f32(512) for _ in range(2)]
        pstT = [bank(k).bitcast(BF16).rearrange("p (c t) -> p c t", c=8) for k in (0, 1)]
        psK = [bank(2), bank(3)]
        psS = bank(4)
        psV = [bank(5), bank(6)]

        for wi, (wbf, c0) in enumerate([(wk_bf, 2048), (wv_bf, 2560)]):
            P.dma("sp", lambda e, c0=c0: e.dma_start(out=wst, in_=w_in_v[:, :, c0:c0 + 512]), writes=["wst"])
            for c in range(8):
                P.op("dve", lambda e, c=c, wbf=wbf: e.tensor_scalar(out=wbf[:, c, :], in0=wst[:, c, :], scalar1=ga2[:, c:c + 1], scalar2=None, op0=ALU.mult),
                     reads=["wst", "ga2"], writes=["wkv%d_%d" % (wi, c)])
        wk_keys = ["wkv0_%d" % c for c in range(8)]
        wv_keys = ["wkv1_%d" % c for c in range(8)]

        CUT = int(os.environ.get("KCUT", "0"))
        if CUT == 1:
            finish_early(); return nc
        tiles = [("h", 0, 0)] + [("t", tg, r) for tg in range(8) for r in range(4)]

        def stageA(t):
            kind, tg, r = tiles[t]
            b = t % 2
            if kind == "h":
                P.dma("sp", lambda e: e.dma_start(out=xs[b][0:32, :], in_=xh), writes=["xs%d" % b])
                rmsnorm_A(xs[b], "xs%d" % b, 32, xn[b], "xn%d" % b, junk)
            else:
                kt = 4 * tg + r
                P.dma("sp", lambda e, kt=kt, b=b: e.dma_start(out=xs[b], in_=xk[kt * 128:(kt + 1) * 128, :]), writes=["xs%d" % b])
                rmsnorm_A(xs[b], "xs%d" % b, 128, xn[b], "xn%d" % b, junk)

        def stageB(t):
            kind, tg, r = tiles[t]
            b = t % 2
            if kind == "h":
                rmsnorm_B(32, xn[b], "xn%d" % b, [(hnTo[:, :, 2048:2080], "hnTo_h", "act")], pstT[b], "pstT%d" % b)
                return
            g = hng[tg % 2]
            gk = "hng%d" % (tg % 2)
            kt = 4 * tg + r
            dsts = [(g[:, :, r * 128:(r + 1) * 128], gk + "_%d" % r, "act")]
            if r % 2 == 0:
                i = kt // 2
                dsts.append((hnTo[:, :, i * 128:(i + 1) * 128], "hnTo_%d" % i, "act"))
            rmsnorm_B(128, xn[b], "xn%d" % b, dsts, pstT[b], "pstT%d" % b)

        def kv_group(tg):
            g = hng[tg % 2]
            gk = "hng%d" % (tg % 2)
            gkeys = [gk + "_%d" % r for r in range(4)]
            for h in range(4):
                pb = (tg * 4 + h) % 2
                P.op("pe", [lambda e, c=c, h=h, pb=pb, g=g: e.matmul(psK[pb], lhsT=wk_bf[:, c, h * 128:(h + 1) * 128], rhs=g[:, c, :], start=(c == 0), stop=(c == 7)) for c in range(8)],
                     reads=gkeys + wk_keys, writes=["psK%d" % pb])
                norm_feat(psK[pb], "psK%d" % pb, gk2, "gk2", kT[:, h, tg * 512:(tg + 1) * 512], "kT", sqb[pb], "sqb%d" % pb, psS, "psS", srt[pb], "srt%d" % pb)
            for r in range(4):
                kt = 4 * tg + r
                pb = r % 2
                P.op("pe", [lambda e, c=c, r=r, pb=pb, g=g: e.matmul(psV[pb], lhsT=g[:, c, r * 128:(r + 1) * 128], rhs=wv_bf[:, c, :], start=(c == 0), stop=(c == 7)) for c in range(8)],
                     reads=[gk + "_%d" % r] + wv_keys, writes=["psV%d" % pb])
                P.op("act", lambda e, kt=kt, pb=pb: e.copy(out=VA[:, kt, :, 0:128], in_=psV[pb].rearrange("p (h v) -> p h v", h=4)), reads=["psV%d" % pb], writes=["VA"])

        stageA(0)
        for t in range(len(tiles)):
            if t + 1 < len(tiles):
                stageA(t + 1)
            stageB(t)
            kind, tg, r = tiles[t]
            if kind == "t" and r == 3:
                kv_group(tg)
        P.barrier()

        if limit == 1:
            finish_early()
            return nc
        R_B = R_A + 8320
        B2 = Bump(R_B, NCOL)
        qT = B2.bf(4 * 2048).rearrange("p (h t) -> p h t", h=4)
        mixT = B2.bf(8 * 2048).rearrange("p (c t) -> p c t", c=8)
        R_C = B2.p
        _w = B2.f32(3072).rearrange("p (j c n) -> p j c n", j=3, c=8)
        wstg = [_w, _w]
        wblk = [B2.bf(3072).rearrange("p (j c n) -> p j c n", j=3, c=8) for _ in range(2)]
        sqb2 = [B2.bf(512) for _ in range(2)]
        srt2 = [B2.f32(512) for _ in range(2)]
        csb = [B2.f32(512) for _ in range(2)]
        ub = [B2.f32(520).rearrange("p (b t) -> p b t", b=4) for _ in range(2)]
        yb = [B2.f32(512) for _ in range(2)]
        uh = B2.f32(128).rearrange("p (c t) -> p c t", c=4)
        hcs = B2.f32(32)
        psA = [bank(0), bank(1)]
        psB = [bank(2), bank(3)]
        psC = [bank(4), bank(5)]
        psS2 = bank(6)
        psH = bank(7)

        jobs = [("q", h, [1536 + h * 128]) for h in range(4)] + [("c", cc, [cc * 128, 512 + cc * 128, 1024 + cc * 128]) for cc in range(4)]
        wkeys = {}

        def load_job(ji):
            kind, idx, cols = jobs[ji]
            b = ji % 2
            keys = []
            for j, c0 in enumerate(cols):
                P.dma("sp", lambda e, b=b, j=j, c0=c0: e.dma_start(out=wstg[b][:, j], in_=w_in_v[:, :, c0:c0 + 128]), writes=["wstg_%d" % j])
                P.op("dve", [lambda e, b=b, j=j, c=c: e.tensor_scalar(out=wblk[b][:, j, c, :], in0=wstg[b][:, j, c, :], scalar1=ga2[:, c:c + 1], scalar2=None, op0=ALU.mult) for c in range(8)],
                     reads=["wstg_%d" % j, "ga2"], writes=["wblk%d_%d" % (b, j)])
                keys.append("wblk%d_%d" % (b, j))
            wkeys[ji] = keys

        load_job(0)
        cnt2 = 0
        for ji, (kind, idx, cols) in enumerate(jobs):
            if ji + 1 < len(jobs):
                load_job(ji + 1)
            b = ji % 2
            wb = wblk[b]
            if kind == "q":
                h = idx
                for tg in range(4):
                    pb = cnt2 % 2
                    cnt2 += 1
                    P.op("pe", [lambda e, c=c, pb=pb, tg=tg, wb=wb: e.matmul(psA[pb], lhsT=wb[:, 0, c, :], rhs=hnTo[:, c, tg * 512:(tg + 1) * 512], start=(c == 0), stop=(c == 7)) for c in range(8)],
                         reads=wkeys[ji], writes=["psA%d" % pb])
                    norm_feat(psA[pb], "psA%d" % pb, gq2, "gq2", qT[:, h, tg * 512:(tg + 1) * 512], "qT", sqb2[pb], "sqb2%d" % pb, psS2, "psS2", srt2[pb], "srt2%d" % pb)
            else:
                cc = idx
                P.op("pe", [lambda e, c=c, wb=wb: e.matmul(psH[:, 0:32], lhsT=wb[:, 0, c, :], rhs=hnTo[:, c, 2048:2080], start=(c == 0), stop=(c == 7)) for c in range(8)],
                     reads=wkeys[ji], writes=["psHx"])
                P.op("pe", [lambda e, c=c, wb=wb: e.matmul(psH[:, 64:96], lhsT=wb[:, 2, c, :], rhs=hnTo[:, c, 2048:2080], start=(c == 0), stop=(c == 7)) for c in range(8)],
                     reads=wkeys[ji], writes=["psHc"])
                P.op("act", lambda e: e.copy(out=hcs, in_=psH[:, 64:96]), reads=["psHc"], writes=["hcs"])
                P.op("dve", lambda e, cc=cc: e.tensor_tensor(out=uh[:, cc, :], in0=psH[:, 0:32], in1=hcs, op=ALU.mult), reads=["psHx", "hcs"], writes=["uh%d" % cc])
                for tg in range(4):
                    pb = cnt2 % 2
                    cnt2 += 1
                    tsl = slice(tg * 512, (tg + 1) * 512)
                    for j, (pp, nm) in enumerate([(psA, "psA"), (psB, "psB"), (psC, "psC")]):
                        P.op("pe", [lambda e, c=c, pb=pb, j=j, pp=pp, wb=wb, tsl=tsl: e.matmul(pp[pb], lhsT=wb[:, j, c, :], rhs=hnTo[:, c, tsl], start=(c == 0), stop=(c == 7)) for c in range(8)],
                             reads=wkeys[ji], writes=["%s%d" % (nm, pb)])
                    u = ub[pb]
                    y = yb[pb]
                    y3 = y.rearrange("p (b t) -> p b t", b=4)
                    P.op("act", lambda e, pb=pb: e.copy(out=csb[pb], in_=psC[pb]), reads=["psC%d" % pb], writes=["csb%d" % pb])
                    P.op("dve", lambda e, pb=pb, u=u: e.tensor_tensor(out=u[:, :, 2:130], in0=psA[pb].rearrange("p (b t) -> p b t", b=4), in1=csb[pb].rearrange("p (b t) -> p b t", b=4), op=ALU.mult),
                         reads=["psA%d" % pb, "csb%d" % pb], writes=["u%d" % pb])
                    P.op("pool", lambda e, u=u, cc=cc, tg=tg: e.tensor_copy(out=u[:, :, 0:2], in_=uh[:, cc, tg * 8:(tg + 1) * 8].rearrange("p (b t) -> p b t", b=4)),
                         reads=["uh%d" % cc], writes=["uhalo%d" % pb])
                    P.op("dve", lambda e, u=u, y3=y3, cc=cc: e.tensor_scalar(out=y3, in0=u[:, :, 0:128], scalar1=cw[0][:, cc:cc + 1], scalar2=None, op0=ALU.mult),
                         reads=["u%d" % pb, "uhalo%d" % pb, "convw0"], writes=["y%d" % pb])
                    for k in (1, 2):
                        P.op("dve", lambda e, u=u, y3=y3, cc=cc, k=k: e.scalar_tensor_tensor(out=y3, in0=u[:, :, k:k + 128], scalar=cw[k][:, cc:cc + 1], in1=y3, op0=ALU.mult, op1=ALU.add),
                             reads=["u%d" % pb, "uhalo%d" % pb, "y%d" % pb, "convw%d" % k], writes=["y%d" % pb])
                    P.op("dve", lambda e, y=y, pb=pb: e.tensor_tensor(out=y, in0=y, in1=psB[pb], op=ALU.mult), reads=["y%d" % pb, "psB%d" % pb], writes=["y%d" % pb])
                    norm_feat(y, "y%d" % pb, None, None, mixT[:, cc, tsl], "mixT", sqb2[pb], "sqb2%d" % pb, psS2, "psS2", srt2[pb], "srt2%d" % pb)
        P.barrier()

        if limit == 2:
            finish_early()
            return nc
        B3 = Bump(R_C, NCOL)
        PT = [B3.bf(512) for _ in range(4)]
        r0b = B3.f32(512)
        r1b = B3.f32(512)
        of_ = B3.f32(512)
        tf_ = B3.f32(512)
        sq3 = B3.bf(512)
        srt3 = B3.f32(512)
        onesB = B3.bf(128)
        P.op("pool", lambda e: e.memset(onesB, 1.0), writes=["onesB"])
        R_D = B3.p
        B3b = Bump(R_B - 8192, R_B)
        wo_bf = B3b.bf(8 * 1024).rearrange("p (c n) -> p c n", c=8)
        wost = B3b.f32(4096).rearrange("p (c n) -> p c n", c=8)
        w_out_v = w_out.rearrange("(c p) n -> p c n", p=128)
        for half in range(2 if os.environ.get('KV') != 'C' else 0):
            P.dma("sp", lambda e, half=half: e.dma_start(out=wost, in_=w_out_v[:, :, half * 512:(half + 1) * 512]), writes=["wost"])
            for c in range(8):
                P.op("dve", lambda e, c=c, half=half: e.tensor_scalar(out=wo_bf[:, c, half * 512:(half + 1) * 512], in0=wost[:, c, :], scalar1=wosc[:, c:c + 1], scalar2=None, op0=ALU.mult),
                     reads=["wost", "wosc_a"] + ["wosc%d" % k for k in range(4, 8)], writes=["wo_bf%d_%d" % (half, c)])
        wo_keys = ["wo_bf%d_%d" % (hf, c) for hf in range(2) for c in range(8)]

        psST = [[bank(4), bank(5)], [bank(6), bank(7)]]
        psOT = [bank(0), bank(1)]
        psSS = [bank(2), bank(3)]
        psN = bank(7)
        psNkey = "psST1_1"

        steps = []
        for G in range(4):
            for h in range(4):
                for j in range(8 * G + 8):
                    steps.append((G, h, j))
        nsteps = len(steps)

        def geom(G, j):
            pj = j // 2
            i0 = max(pj, 4 * G)
            nq = (4 * G + 4 - i0) * 128
            return pj, i0, nq

        def emit_ST(si):
            G, h, j = steps[si]
            pj, i0, nq = geom(G, j)
            q0 = i0 * 128
            sb = si % 2
            P.op("pe", [lambda e, c=c: e.matmul(psST[sb][c][:, 0:nq], lhsT=kT[c * 64:(c + 1) * 64, h, j * 128:(j + 1) * 128], rhs=qT[c * 64:(c + 1) * 64, h, q0:q0 + nq], start=True, stop=True) for c in range(2)],
                 reads=["kT", "qT"], writes=["psST%d_0" % sb, "psST%d_1" % sb])

        def emit_exp_pv(si):
            G, h, j = steps[si]
            pj, i0, nq = geom(G, j)
            qoff = 512 - nq
            sb = si % 2
            special = pj >= 4 * G
            allk = []
            pts = []
            for c in range(2):
                pt = PT[sb * 2 + c]
                ptk = "PT%d" % (sb * 2 + c)
                src = psST[sb][c]
                sk_ = "psST%d_%d" % (sb, c)
                if special and j % 2 == 1:
                    P.op("act", lambda e, pt=pt, src=src: e.activation(out=pt[:, 0:128], in_=src[:, 0:128], func=AF.Exp, bias=obias[:, pj % 2:pj % 2 + 1], scale=1.0),
                         reads=[sk_, "obias"], writes=[ptk])
                    if nq > 128:
                        P.op("act", lambda e, pt=pt, src=src: e.activation(out=pt[:, 128:nq], in_=src[:, 128:nq], func=AF.Exp), reads=[sk_], writes=[ptk + "r"])
                        allk += [ptk, ptk + "r"]
                    else:
                        allk += [ptk]
                else:
                    P.op("act", lambda e, pt=pt, src=src: e.activation(out=pt[:, 0:nq], in_=src[:, 0:nq], func=AF.Exp), reads=[sk_], writes=[ptk, ptk + "r"])
                    allk += [ptk, ptk + "r"]
                    if special:
                        P.op("pool", lambda e, pt=pt: e.tensor_tensor(out=pt[:, 0:128], in0=pt[:, 0:128], in1=triB, op=ALU.mult), reads=[ptk, "triB"], writes=[ptk])
                pts.append(pt)
            last = (j == 8 * G + 7)
            fns = []
            for c in range(2):
                fns.append(lambda e, c=c: e.matmul(psOT[c][:, qoff:512], lhsT=VA[:, j, h, 0:128], rhs=pts[c][:, 0:nq], start=(j == 0), stop=last))
                fns.append(lambda e, c=c: e.matmul(psSS[c][:, qoff:512], lhsT=onesB, rhs=pts[c][:, 0:nq], start=(j == 0), stop=last))
            P.op("pe", fns, reads=allk + ["VA", "onesB"], writes=["psOT0", "psSS0", "psOT1", "psSS1"])

        def emit_final_a(G, h):
            P.op("dve", lambda e: e.reciprocal(out=r0b, in_=psSS[0]), reads=["psSS0"], writes=["r0b"])
            P.op("dve", lambda e: e.reciprocal(out=r1b, in_=psSS[1]), reads=["psSS1"], writes=["r1b"])
            P.op("dve", lambda e: e.tensor_tensor(out=of_, in0=psOT[0], in1=r0b, op=ALU.mult), reads=["psOT0", "r0b"], writes=["of"])
            P.op("dve", lambda e: e.tensor_tensor(out=tf_, in0=psOT[1], in1=r1b, op=ALU.mult), reads=["psOT1", "r1b"], writes=["tf"])
            P.op("dve", lambda e: e.scalar_tensor_tensor(out=of_, in0=tf_, scalar=neglam, in1=of_, op0=ALU.mult, op1=ALU.add), reads=["tf", "of", "neglam"], writes=["of"])

        def emit_final_b(G, h):
            P.op("act", lambda e: e.activation(out=sq3, in_=of_, func=AF.Square), reads=["of"], writes=["sq3"])
            P.op("pe", lambda e: e.matmul(psN, lhsT=onesB, rhs=sq3, start=True, stop=True), reads=["sq3", "onesB"], writes=[psNkey])
            P.op("act", lambda e: e.activation(out=srt3, in_=psN, func=AF.Sqrt, scale=1.0 / 128, bias=EPS), reads=[psNkey], writes=["srt3"])
            P.op("dve", lambda e: e.reciprocal(out=srt3, in_=srt3), reads=["srt3"], writes=["srt3"])
            P.op("dve", lambda e: e.tensor_tensor(out=mixT[:, 4 + h, G * 512:(G + 1) * 512], in0=of_, in1=srt3, op=ALU.mult), reads=["of", "srt3"], writes=["mixT"])

        pending = []
        emit_ST(0)
        for si in range(nsteps):
            if si + 1 < nsteps:
                emit_ST(si + 1)
            emit_exp_pv(si)
            G, h, j = steps[si]
            if pending and si >= pending[0][0]:
                _, pg, ph = pending.pop(0)
                emit_final_b(pg, ph)
            if j == 8 * G + 7:
                emit_final_a(G, h)
                pending.append((si + 4, G, h))
        for _, pg, ph in pending:
            emit_final_b(pg, ph)
        P.barrier()

        if limit == 3:
            finish_early()
            return nc
        B4 = Bump(R_KT, R_A)
        hn2T = B4.bf(8 * 2048).rearrange("p (c t) -> p c t", c=8)
        combT = [B4.bf(2048) for _ in range(2)]
        R_E = B4.p
        B4 = Bump(R_D, NCOL)
        xs4 = [B4.f32(1024) for _ in range(2)]
        ht = [B4.f32(1024) for _ in range(2)]
        hn2 = [B4.bf(1024) for _ in range(2)]
        junk4 = B4.bf(1024)
        wrst = B4.f32(8 * 36).rearrange("p (c n) -> p c n", c=8)
        wr_bf = B4.bf(8 * 36).rearrange("p (c n) -> p c n", c=8)
        Lb = [B4.f32(36) for _ in range(2)]
        rt = [B4.f32(160) for _ in range(2)]
        chl_all = B4.bf(16 * 64).rearrange("p (i k) -> p i k", i=16)
        psO = [(bank(0), bank(1)), (bank(4), bank(5))]
        psT4 = [bank(2).bitcast(BF16).rearrange("p (c t) -> p c t", c=8) for k in (0, 1)]
        psL = [bank(3)[:, 0:36], bank(3)[:, 0:36]]
        psCm = [bank(6).bitcast(BF16)[:, 0:256], bank(7).bitcast(BF16)[:, 0:256]]
        psMT = bank(5)[:, 0:256].bitcast(BF16)

        P.dma("sp", lambda e: e.dma_start(out=wrst[:, :, 0:4], in_=wrg.rearrange("(c p) n -> p c n", p=128), allow_slow_non_contiguous=True), writes=["wrst_a"])
        P.dma("sp", lambda e: e.dma_start(out=wrst[:, :, 4:36], in_=wre.rearrange("(c p) n -> p c n", p=128), allow_slow_non_contiguous=True), writes=["wrst_b"])
        P.op("dve", [lambda e, c=c: e.tensor_scalar(out=wr_bf[:, c, :], in0=wrst[:, c, :], scalar1=gf2[:, c:c + 1], scalar2=None, op0=ALU.mult) for c in range(8)],
             reads=["wrst_a", "wrst_b", "gf2"], writes=["wr_bf"])
        for q4 in range(4):
            P.dma("sp", lambda e, q4=q4: e.dma_start(out=xs4[0][0:32, :], in_=sel_d[:, q4 * 1024:(q4 + 1) * 1024]), writes=["xs4_0"])
            P.op("pool", lambda e, q4=q4: e.tensor_copy(out=selB[0:32, q4 * 1024:(q4 + 1) * 1024], in_=xs4[0][0:32, :]), reads=["xs4_0"], writes=["selB"])

        BIG = 10000.0
        def front(i):
            b = i % 2
            P.dma("sp", lambda e, i=i, b=b: e.dma_start(out=xs4[b], in_=xk[(2 * i) * 128:(2 * i + 1) * 128, :]), writes=["xs4_%d" % b])
            for half in range(2):
                P.op("pe", [lambda e, c=c, half=half, i=i, b=b: e.matmul(psO[b][half], lhsT=mixT[:, c, i * 128:(i + 1) * 128], rhs=wo_bf[:, c, half * 512:(half + 1) * 512], start=(c == 0), stop=(c == 7)) for c in range(8)],
                     reads=["mixT"] + wo_keys, writes=["psO%d_%d" % (b, half)])
                P.op("dve", lambda e, half=half, b=b: e.tensor_tensor(out=ht[b][:, half * 512:(half + 1) * 512], in0=psO[b][half], in1=xs4[b][:, half * 512:(half + 1) * 512], op=ALU.add),
                     reads=["psO%d_%d" % (b, half), "xs4_%d" % b], writes=["ht%d_%d" % (b, half)])
            htk = ["ht%d_0" % b, "ht%d_1" % b]
            P.dma("pool", lambda e, i=i, b=b: e.dma_start(out=hscr[i * 128:(i + 1) * 128, :], in_=ht[b]), reads=htk, writes=["hscr"])
            ss, rs, sk = newstat()
            P.op("act", lambda e, b=b, ss=ss: e.activation(out=junk4, in_=ht[b], func=AF.Square, accum_out=ss), reads=htk + ["stats"], writes=[sk, "junk4"])
            P.op("act", lambda e, ss=ss, rs=rs: e.activation(out=rs, in_=ss, func=AF.Sqrt, scale=1.0 / 1024, bias=EPS), reads=[sk], writes=[sk + "r"])
            P.op("dve", lambda e, rs=rs: e.reciprocal(out=rs, in_=rs), reads=[sk + "r"], writes=[sk + "r"])
            P.op("dve", lambda e, b=b, rs=rs: e.tensor_scalar(out=hn2[b], in0=ht[b], scalar1=rs, scalar2=None, op0=ALU.mult), reads=htk + [sk + "r"], writes=["hn2_%d" % b])

        def back(i):
            b = i % 2
            P.op("pe", [lambda e, c=c, b=b: e.transpose(out=psT4[b][:, c, :], in_=hn2[b][:, c * 128:(c + 1) * 128], identity=identB) for c in range(8)],
                 reads=["hn2_%d" % b, "identB"], writes=["psT4"])
            P.op("act", lambda e, b=b, i=i: e.copy(out=hn2T[:, :, i * 128:(i + 1) * 128], in_=psT4[b]), reads=["psT4"], writes=["hn2T_%d" % i])
            P.op("pe", [lambda e, c=c, b=b, i=i: e.matmul(psL[b], lhsT=hn2T[:, c, i * 128:(i + 1) * 128], rhs=wr_bf[:, c, :], start=(c == 0), stop=(c == 7)) for c in range(8)],
                 reads=["hn2T_%d" % i, "wr_bf"], writes=["psL"])
            L = Lb[b]
            T = rt[b]
            lk = "L%d" % b
            tk = "rt%d_" % b
            P.op("act", lambda e, L=L, b=b: e.copy(out=L, in_=psL[b]), reads=["psL"], writes=[lk])
            mg, nmg, eg, sg, gg, oh, pen = T[:, 0:1], T[:, 1:2], T[:, 2:6], None, T[:, 7:8], T[:, 8:12], T[:, 12:16]
            LM = T[:, 16:48]
            m1, mk1, LM2, m2, mk2 = T[:, 48:49], T[:, 49:81], T[:, 81:113], T[:, 113:114], T[:, 114:146]
            dd, ed, den, w1, w2 = T[:, 146:147], T[:, 147:148], T[:, 148:149], T[:, 149:150], T[:, 150:151]
            comb = T[:, 16:48]
            comb = T[:, 81:113]
            ssg, _, skg = newstat()
            P.op("dve", lambda e, L=L, mg=mg: e.reduce_max(out=mg, in_=L[:, 0:4], axis=AX.X), reads=[lk], writes=[tk + "mg"])
            P.op("dve", lambda e, mg=mg, nmg=nmg: e.tensor_scalar(out=nmg, in0=mg, scalar1=-1.0, scalar2=None, op0=ALU.mult), reads=[tk + "mg"], writes=[tk + "nmg"])
            P.op("act", lambda e, L=L, eg=eg, nmg=nmg, ssg=ssg: e.activation(out=eg, in_=L[:, 0:4], func=AF.Exp, bias=nmg, scale=1.0, accum_out=ssg), reads=[lk, tk + "nmg", "stats"], writes=[tk + "eg", skg])
            P.op("dve", lambda e, gg=gg, ssg=ssg: e.reciprocal(out=gg, in_=ssg), reads=[skg], writes=[tk + "gg"])
            P.op("dve", lambda e, L=L, oh=oh, mg=mg: e.tensor_scalar(out=oh, in0=L[:, 0:4], scalar1=mg, scalar2=None, op0=ALU.is_ge), reads=[lk, tk + "mg"], writes=[tk + "oh"])
            P.op("dve", lambda e, oh=oh, pen=pen: e.tensor_scalar(out=pen, in0=oh, scalar1=BIG, scalar2=-BIG, op0=ALU.mult, op1=ALU.add), reads=[tk + "oh"], writes=[tk + "pen"])
            for g in range(4):
                P.op("dve", lambda e, L=L, LM=LM, pen=pen, g=g: e.tensor_scalar(out=LM[:, g * 8:(g + 1) * 8], in0=L[:, 4 + g * 8:12 + g * 8], scalar1=pen[:, g:g + 1], scalar2=None, op0=ALU.add),
                     reads=[lk, tk + "pen"], writes=[tk + "LM"])
            P.op("dve", lambda e, LM=LM, m1=m1: e.reduce_max(out=m1, in_=LM, axis=AX.X), reads=[tk + "LM"], writes=[tk + "m1"])
            P.op("dve", lambda e, LM=LM, m1=m1, mk1=mk1: e.tensor_scalar(out=mk1, in0=LM, scalar1=m1, scalar2=None, op0=ALU.is_ge), reads=[tk + "LM", tk + "m1"], writes=[tk + "mk1"])
            P.op("dve", lambda e, LM=LM, mk1=mk1, LM2=LM2: e.scalar_tensor_tensor(out=LM2, in0=mk1, scalar=-BIG, in1=LM, op0=ALU.mult, op1=ALU.add), reads=[tk + "LM", tk + "mk1"], writes=[tk + "LM2"])
            P.op("dve", lambda e, LM2=LM2, m2=m2: e.reduce_max(out=m2, in_=LM2, axis=AX.X), reads=[tk + "LM2"], writes=[tk + "m2"])
            P.op("dve", lambda e, LM2=LM2, m2=m2, mk2=mk2: e.tensor_scalar(out=mk2, in0=LM2, scalar1=m2, scalar2=None, op0=ALU.is_ge), reads=[tk + "LM2", tk + "m2"], writes=[tk + "mk2"])
            P.op("dve", lambda e, dd=dd, m1=m1, m2=m2: e.tensor_tensor(out=dd, in0=m2, in1=m1, op=ALU.subtract), reads=[tk + "m1", tk + "m2"], writes=[tk + "dd"])
            P.op("act", lambda e, dd=dd, ed=ed: e.activation(out=ed, in_=dd, func=AF.Exp), reads=[tk + "dd"], writes=[tk + "ed"])
            P.op("dve", lambda e, ed=ed, den=den: e.tensor_scalar(out=den, in0=ed, scalar1=1.0, scalar2=None, op0=ALU.add), reads=[tk + "ed"], writes=[tk + "den"])
            P.op("dve", lambda e, den=den, w1=w1: e.reciprocal(out=w1, in_=den), reads=[tk + "den"], writes=[tk + "w1"])
            P.op("dve", lambda e, w1=w1, gg=gg: e.tensor_tensor(out=w1, in0=w1, in1=gg, op=ALU.mult), reads=[tk + "w1", tk + "gg"], writes=[tk + "w1"])
            P.op("dve", lambda e, w1=w1, w2=w2, ed=ed: e.tensor_tensor(out=w2, in0=w1, in1=ed, op=ALU.mult), reads=[tk + "w1", tk + "ed"], writes=[tk + "w2"])
            P.op("dve", lambda e, comb=comb, mk2=mk2, w2=w2: e.tensor_scalar(out=comb, in0=mk2, scalar1=w2, scalar2=None, op0=ALU.mult), reads=[tk + "mk2", tk + "w2", tk + "LM2"], writes=[tk + "LM2"])
            P.op("dve", lambda e, comb=comb, mk1=mk1, w1=w1: e.scalar_tensor_tensor(out=comb, in0=mk1, scalar=w1, in1=comb, op0=ALU.mult, op1=ALU.add), reads=[tk + "mk1", tk + "w1", tk + "LM2"], writes=[tk + "comb"])
            ch = chl_all[:, i, :]
            P.op("dve", lambda e, comb=comb, ch=ch: e.tensor_copy(out=ch[:, 0:32], in_=comb), reads=[tk + "comb"], writes=["chl%d_h" % i])
            P.op("dve", lambda e, comb=comb, ch=ch: e.tensor_tensor(out=ch[:, 32:64], in0=comb, in1=ch[:, 0:32], op=ALU.subtract), reads=[tk + "comb", "chl%d_h" % i], writes=["chl%d_l" % i])

        front(0)
        for i in range(16):
            if i + 1 < 16:
                front(i + 1)
            back(i)
        for i in range(16):
            b = i % 2
            ch = chl_all[:, i, :]
            P.op("pe", [lambda e, ch=ch, b=b: e.transpose(out=psCm[b][0:32, 0:128], in_=ch[:, 0:32], identity=identB),
                        lambda e, ch=ch, b=b: e.transpose(out=psCm[b][0:32, 128:256], in_=ch[:, 32:64], identity=identB)],
                 reads=["chl%d_h" % i, "chl%d_l" % i, "identB"], writes=["psCm%d" % b])
            P.op("act", lambda e, b=b, i=i: e.copy(out=combT[0][0:32, i * 128:(i + 1) * 128], in_=psCm[b][0:32, 0:128]), reads=["psCm%d" % b], writes=["combT0_%d" % (i // 4)])
            P.op("act", lambda e, b=b, i=i: e.copy(out=combT[1][0:32, i * 128:(i + 1) * 128], in_=psCm[b][0:32, 128:256]), reads=["psCm%d" % b], writes=["combT1_%d" % (i // 4)])
        P.barrier()

        if limit == 4:
            finish_early()
            return nc
        B5 = Bump(R_E, NCOL)
        yacc = B5.f32(16 * 1024).rearrange("p (i n) -> p i n", i=16)
        wgb = [B5.bf(8 * 512).rearrange("p (c n) -> p c n", c=8) for _ in range(2)]
        wub = [B5.bf(8 * 512).rearrange("p (c n) -> p c n", c=8) for _ in range(2)]
        wdb = [B5.bf(4 * 1024).rearrange("p (c n) -> p c n", c=4) for _ in range(2)]
        wst5 = [B5.f32(2048).rearrange("p (c n) -> p c n", c=4) for _ in range(2)]
        actT = [[B5.bf(512) for _ in range(4)] for _ in range(2)]
        sb5 = [B5.f32(512) for _ in range(2)]
        tb5 = [B5.f32(512) for _ in range(2)]
        cbs = [B5.f32(512) for _ in range(2)]
        psG = [bank(0), bank(1)]
        psU = [bank(2), bank(3)]
        psY = [bank(4), bank(5)]
        psCB = bank(6)

        piece = [0]

        def load_expert(ex, which):
            wb = ex % 2
            srcs = []
            gv = weg[ex].rearrange("(c p) n -> p c n", p=128)
            uv = weu[ex].rearrange("(c p) n -> p c n", p=128)
            dv = wed[ex].rearrange("(c p) n -> p c n", p=128)
            srcs.append((gv[:, 0:4, :], wgb[wb], 0, "g", True))
            srcs.append((gv[:, 4:8, :], wgb[wb], 4, "g", True))
            srcs.append((uv[:, 0:4, :], wub[wb], 0, "u", True))
            srcs.append((uv[:, 4:8, :], wub[wb], 4, "u", True))
            srcs.append((dv[:, :, 0:512], wdb[wb], 0, "d", False))
            srcs.append((dv[:, :, 512:1024], wdb[wb], 512, "d", False))
            for (src, dst, off, nm, scaled) in [srcs[k] for k in which]:
                sbi = piece[0] % 2
                piece[0] += 1
                P.dma("sp", lambda e, src=src, sbi=sbi: e.dma_start(out=wst5[sbi], in_=src), writes=["wst5_%d" % sbi])
                if scaled:
                    for c in range(4):
                        P.op("act", lambda e, c=c, dst=dst, off=off, sbi=sbi: e.mul(out=dst[:, off + c, :], in_=wst5[sbi][:, c, :], mul=gf2[:, off + c:off + c + 1]),
                             reads=["wst5_%d" % sbi, "gf2"], writes=["w%s%d_%d" % (nm, wb, off + c)])
                else:
                    P.op("dve", lambda e, dst=dst, off=off, sbi=sbi: e.tensor_copy(out=dst[:, :, off:off + 512], in_=wst5[sbi]),
                         reads=["wst5_%d" % sbi], writes=["wd%d_%d" % (wb, off)])

        def gu_keys(wb):
            return ["wg%d_%d" % (wb, c) for c in range(8)] + ["wu%d_%d" % (wb, c) for c in range(8)]

        def emit_GU(ex, tb, ab):
            wb = ex % 2
            tsl = slice(tb * 512, (tb + 1) * 512)
            cb = (ex * 4 + tb) % 2
            P.op("pe", [lambda e: e.matmul(psCB, lhsT=selB[0:32, ex * 128:(ex + 1) * 128], rhs=combT[0][0:32, tsl], start=True, stop=False),
                        lambda e: e.matmul(psCB, lhsT=selB[0:32, ex * 128:(ex + 1) * 128], rhs=combT[1][0:32, tsl], start=False, stop=True)],
                 reads=["selB", "combT0_%d" % tb, "combT1_%d" % tb], writes=["psCB"])
            P.op("act", lambda e: e.copy(out=cbs[cb], in_=psCB), reads=["psCB"], writes=["cbs%d" % cb])
            for fc in range(4):
                pb = fc % 2
                P.op("pe", [lambda e, c=c, fc=fc, pb=pb: e.matmul(psG[pb], lhsT=wgb[wb][:, c, fc * 128:(fc + 1) * 128], rhs=hn2T[:, c, tsl], start=(c == 0), stop=(c == 7)) for c in range(8)],
                     reads=["wg%d_%d" % (wb, c) for c in range(8)], writes=["psG%d" % pb])
                P.op("pe", [lambda e, c=c, fc=fc, pb=pb: e.matmul(psU[pb], lhsT=wub[wb][:, c, fc * 128:(fc + 1) * 128], rhs=hn2T[:, c, tsl], start=(c == 0), stop=(c == 7)) for c in range(8)],
                     reads=["wu%d_%d" % (wb, c) for c in range(8)], writes=["psU%d" % pb])
                P.op("act", lambda e, pb=pb: e.activation(out=sb5[pb], in_=psG[pb], func=AF.Silu), reads=["psG%d" % pb], writes=["sb5_%d" % pb])
                P.op("dve", lambda e, pb=pb: e.tensor_tensor(out=tb5[pb], in0=sb5[pb], in1=psU[pb], op=ALU.mult), reads=["sb5_%d" % pb, "psU%d" % pb], writes=["tb5_%d" % pb])
                P.op("pool", lambda e, pb=pb, fc=fc: e.tensor_tensor(out=actT[ab][fc], in0=tb5[pb], in1=cbs[cb], op=ALU.mult), reads=["tb5_%d" % pb, "cbs%d" % cb], writes=["actT%d_%d" % (ab, fc)])

        dcount = [0]

        def emit_DOWN(ex, tb, ab):
            wb = ex % 2
            for tbl in range(4):
                ti = tb * 4 + tbl
                for half in range(2):
                    pb = dcount[0] % 2
                    dcount[0] += 1
                    P.op("pe", [lambda e, fc=fc, pb=pb, tbl=tbl, half=half: e.matmul(psY[pb], lhsT=actT[ab][fc][:, tbl * 128:(tbl + 1) * 128], rhs=wdb[wb][:, fc, half * 512:(half + 1) * 512], start=(fc == 0), stop=(fc == 3)) for fc in range(4)],
                         reads=["actT%d_%d" % (ab, fc) for fc in range(4)] + ["wd%d_0" % wb, "wd%d_512" % wb], writes=["psY%d" % pb])
                    dst = yacc[:, ti, half * 512:(half + 1) * 512]
                    if ex == 0:
                        P.op("dve", lambda e, pb=pb, dst=dst: e.tensor_copy(out=dst, in_=psY[pb]), reads=["psY%d" % pb], writes=["yacc"])
                    else:
                        P.op("dve", lambda e, pb=pb, dst=dst: e.tensor_tensor(out=dst, in0=dst, in1=psY[pb], op=ALU.add), reads=["psY%d" % pb, "yacc"], writes=["yacc"])

        load_expert(0, range(6))
        work = [(ex, tb) for ex in range(n_exp) for tb in range(4)]
        for wi, (ex, tb) in enumerate(work):
            emit_GU(ex, tb, wi % 2)
            if wi > 0:
                pex, ptb = work[wi - 1]
                emit_DOWN(pex, ptb, (wi - 1) % 2)
            if ex + 1 < n_exp and tb < 3:
                load_expert(ex + 1, [2 * tb, 2 * tb + 1])
        pex, ptb = work[-1]
        emit_DOWN(pex, ptb, (len(work) - 1) % 2)
        P.barrier()

        if limit == 5:
            finish_early()
            return nc
        B6 = Bump(R_E + 16384, NCOL)
        hl = [B6.f32(1024) for _ in range(2)]
        ob6 = [B6.f32(1024) for _ in range(2)]
        for i in range(16):
            b = i % 2
            P.dma("sp", lambda e, i=i, b=b: e.dma_start(out=hl[b], in_=hscr[i * 128:(i + 1) * 128, :]), reads=["hscr"], writes=["hl%d" % b])
            eng = "dve" if i % 2 == 0 else "pool"
            P.op(eng, lambda e, i=i, b=b: e.tensor_tensor(out=ob6[b], in0=yacc[:, i, :], in1=hl[b], op=ALU.add), reads=["yacc", "hl%d" % b], writes=["ob6_%d" % b])
            P.dma("pool" if i % 2 == 0 else "sp", lambda e, i=i, b=b: e.dma_start(out=out[i * 128:(i + 1) * 128, :], in_=ob6[b]), reads=["ob6_%d" % b], writes=["out"])
        P.final_wait("pool", ["out"])
        P.final_wait("sp", ["out"])
        P.emit(nc, st)
    return nc


def _own_block(i, half):
    return 2 * i + ((i + half) % 2)


def _host_consts():
    ident = np.eye(128, dtype=np.float32)
    k = np.arange(128)[:, None]
    q = np.arange(128)[None, :]
    tri = (k <= q).astype(np.float32)
    b64 = (k // 64 == q // 64).astype(np.float32)
    consts = np.concatenate([ident, tri, b64], axis=1)
    sel = np.zeros((32, 32, 128), np.float32)
    for e in range(32):
        sel[e, e, :] = 1.0
    return np.ascontiguousarray(consts), np.ascontiguousarray(sel.reshape(32, 4096))


_NC_CACHE = {}


def kernel(x, attn_norm_g, w_in, conv_w, conv_out_g, q_norm_g, k_norm_g,
           lambda_q1, lambda_k1, lambda_q2, lambda_k2, attn_subln_g, w_out,
           ffn_norm_g, w_router_group, w_router_expert, w_exp_gate, w_exp_up, w_exp_down):
    x = np.asarray(x, dtype=np.float32)
    f = lambda a: np.ascontiguousarray(np.asarray(a, dtype=np.float32))
    consts, sel = _host_consts()
    shared = {
        "consts": consts, "sel": sel,
        "attn_norm_g": f(attn_norm_g).reshape(1024), "w_in": f(w_in).reshape(1024, 3072),
        "conv_w": f(conv_w).reshape(3, 512), "conv_out_g": f(conv_out_g).reshape(512),
        "q_norm_g": f(q_norm_g).reshape(64), "k_norm_g": f(k_norm_g).reshape(64),
        "lambda_q1": f(lambda_q1).reshape(64), "lambda_k1": f(lambda_k1).reshape(64),
        "lambda_q2": f(lambda_q2).reshape(64), "lambda_k2": f(lambda_k2).reshape(64),
        "attn_subln_g": f(attn_subln_g).reshape(128), "w_out": f(w_out).reshape(1024, 1024),
        "ffn_norm_g": f(ffn_norm_g).reshape(1024),
        "w_router_group": f(w_router_group).reshape(1024, 4), "w_router_expert": f(w_router_expert).reshape(1024, 32),
        "w_exp_gate": f(w_exp_gate).reshape(32, 1024, 512), "w_exp_up": f(w_exp_up).reshape(32, 1024, 512),
        "w_exp_down": f(w_exp_down).reshape(32, 512, 1024),
    }
    in_maps = []
    for core in range(8):
        b, half = core // 2, core % 2
        xb = x[b].reshape(32, 128, 1024)
        order = []
        halo = np.zeros((16, 2, 1024), np.float32)
        for i in range(16):
            own = _own_block(i, half)
            other = 4 * i + 1 - own
            order += [own, other]
            if own > 0:
                halo[i] = xb[own - 1, 126:128, :]
        xk = np.ascontiguousarray(xb[order].reshape(4096, 1024))
        ob = np.zeros((128, 2), np.float32)
        for par in range(2):
            visible = ((par + half) % 2) == 1
            ob[:, par] = 0.0 if visible else -30000.0
        m = dict(shared)
        m.update({"xk": xk, "xh": np.ascontiguousarray(halo.reshape(32, 1024)), "obias": ob})
        in_maps.append(m)
    if "nc" not in _NC_CACHE:
        _NC_CACHE["nc"] = build_program()
    res = run_bass_kernel_spmd(_NC_CACHE["nc"], in_maps, core_ids=list(range(8)))
    outp = np.empty((4, 32, 128, 1024), np.float32)
    for core in range(8):
        b, half = core // 2, core % 2
        o = np.asarray(res.results[core]["out"]).reshape(16, 128, 1024)
        for i in range(16):
            outp[b, _own_block(i, half)] = o[i]
    return outp.reshape(4, 4096, 1024)
```

```python
import os
import numpy as np
from contextlib import ExitStack
import concourse.bass as bass
import concourse.mybir as mybir
from concourse.bass_utils import run_bass_kernel_spmd

F32 = mybir.dt.float32
BF16 = mybir.dt.bfloat16
AF = mybir.ActivationFunctionType
ALU = mybir.AluOpType
AX = mybir.AxisListType

ENGS = ["pe", "act", "dve", "pool", "sp"]
EPS = 1e-6
NEXP = 32


class Prog:
    def __init__(self):
        self.ops = {e: [] for e in ENGS}
        self.cnt = {}
        self.waited = {e: {} for e in ENGS}
        self.lastw = {}
        self.readers = {}

    def _waits(self, eng, reads, writes):
        ev = []
        for k in reads:
            if k in self.lastw:
                ev.append(self.lastw[k])
        for k in writes:
            if k in self.lastw:
                ev.append(self.lastw[k])
            ev += self.readers.get(k, [])
        need = {}
        for sk, v in ev:
            if sk == "E_pe" and eng == "pe":
                continue
            if self.waited[eng].get(sk, 0) >= v:
                continue
            if need.get(sk, 0) < v:
                need[sk] = v
        for sk, v in need.items():
            self.waited[eng][sk] = v
        return list(need.items())

    def _book(self, me, reads, writes):
        for k in reads:
            self.readers.setdefault(k, []).append(me)
        for k in writes:
            self.lastw[k] = me
            self.readers[k] = []

    def op(self, eng, fns, reads=(), writes=()):
        if not isinstance(fns, (list, tuple)):
            fns = [fns]
        waits = self._waits(eng, reads, writes)
        sk = "E_" + eng
        self.cnt[sk] = self.cnt.get(sk, 0) + 1
        me = (sk, self.cnt[sk])
        self.ops[eng].append((waits, list(fns), (sk, 1)))
        self._book(me, reads, writes)

    def dma(self, eng, fn, reads=(), writes=(), sem=None):
        waits = self._waits(eng, reads, writes)
        sk = "D_" + (sem if sem is not None else writes[0])
        self.cnt[sk] = self.cnt.get(sk, 0) + 16
        me = (sk, self.cnt[sk])
        self.ops[eng].append((waits, [fn], (sk, 16)))
        self._book(me, reads, writes)

    def barrier(self):
        for eng in ENGS:
            need = []
            for sk, v in self.cnt.items():
                if sk == "E_" + eng:
                    continue
                if self.waited[eng].get(sk, 0) >= v:
                    continue
                self.waited[eng][sk] = v
                need.append((sk, v))
            if need:
                self.ops[eng].append((need, [], None))

    def final_wait(self, eng, keys):
        waits = self._waits(eng, keys, [])
        self.ops[eng].append((waits, [], None))

    def emit(self, nc, stack):
        sems = {}
        for sk in self.cnt:
            sems[sk] = stack.enter_context(nc.semaphore(sk))
        block = stack.enter_context(nc.Block())
        prog = self

        def mk(name):
            def f(e):
                for waits, fns, inc in prog.ops[name]:
                    for sk, v in waits:
                        e.wait_ge(sems[sk], v)
                    if not fns:
                        continue
                    for fn in fns[:-1]:
                        fn(e)
                    ins = fns[-1](e)
                    ins.then_inc(sems[inc[0]], inc[1])
            return f

        block.tensor(mk("pe"))
        block.scalar(mk("act"))
        block.vector(mk("dve"))
        block.gpsimd(mk("pool"))
        block.sync(mk("sp"))


def build_program(n_exp=NEXP, limit=99, dump_at=0):
    nc = bass.Bass("TRN2", target_bir_lowering=False)

    def din(name, shape):
        return nc.dram_tensor(name, list(shape), F32, kind="ExternalInput").ap()

    xk = din("xk", [4096, 1024])
    xh = din("xh", [32, 1024])
    obias_d = din("obias", [128, 2])
    consts_d = din("consts", [128, 384])
    sel_d = din("sel", [32, 4096])
    g_attn = din("attn_norm_g", [1024])
    w_in = din("w_in", [1024, 3072])
    conv_w = din("conv_w", [3, 512])
    conv_out_g = din("conv_out_g", [512])
    q_norm_g = din("q_norm_g", [64])
    k_norm_g = din("k_norm_g", [64])
    lq1 = din("lambda_q1", [64])
    lk1 = din("lambda_k1", [64])
    lq2 = din("lambda_q2", [64])
    lk2 = din("lambda_k2", [64])
    subln_g = din("attn_subln_g", [128])
    w_out = din("w_out", [1024, 1024])
    g_ffn = din("ffn_norm_g", [1024])
    wrg = din("w_router_group", [1024, 4])
    wre = din("w_router_expert", [1024, 32])
    weg = din("w_exp_gate", [32, 1024, 512])
    weu = din("w_exp_up", [32, 1024, 512])
    wed = din("w_exp_down", [32, 512, 1024])
    out = nc.dram_tensor("out", [2048, 1024], F32, kind="ExternalOutput").ap()
    hscr = nc.dram_tensor("hscr", [2048, 1024], F32).ap()

    P = Prog()
    with ExitStack() as st:
        NCOL = 52800
        arena = st.enter_context(nc.sbuf_tensor("arena", [128, NCOL], F32))
        ps = st.enter_context(nc.psum_tensor("ps", [128, 4096], F32))

        class Bump:
            def __init__(self, lo, hi):
                self.p, self.hi = lo, hi

            def f32(self, n):
                a = arena[:, self.p:self.p + n]
                self.p += n
                assert self.p <= self.hi, (self.p, self.hi)
                return a

            def bf(self, n):
                assert n % 2 == 0
                return self.f32(n // 2).bitcast(BF16)

        def bank(k, n=512, off=0):
            return ps[:, k * 512 + off:k * 512 + off + n]

        def finish_early():
            ov = out.rearrange("(p a) n -> p (a n)", p=128)
            for q in range(8):
                if dump_at + (q + 1) * 2048 > NCOL:
                    break
                P.dma("sp", lambda e, q=q: e.dma_start(out=ov[:, q * 2048:(q + 1) * 2048], in_=arena[:, dump_at + q * 2048:dump_at + (q + 1) * 2048]), writes=["out"])
            P.final_wait("sp", ["out"])
            P.emit(nc, st)

        C = Bump(0, 3500)
        identF = C.f32(128)
        triF = C.f32(128)
        b64F = C.f32(128)
        identB = C.bf(128)
        triB = C.bf(128)
        b64B = C.bf(128)
        selF_stage = None
        selB = C.bf(4096)
        ga2 = C.f32(8)
        gf2 = C.f32(8)
        cw = [C.f32(4) for _ in range(3)]
        cog = C.f32(4)
        gq2 = C.f32(1)
        gk2 = C.f32(1)
        sublnc = C.f32(1)
        wosc = C.f32(8)
        obias = C.f32(2)
        lamb = C.f32(256).rearrange("p (a d) -> p a d", a=4)
        lamt = C.f32(8)
        neglam = C.f32(1)
        stats = C.f32(256)
        rsc = C.f32(256)
        stat_i = [0]

        def newstat():
            i = stat_i[0]
            stat_i[0] += 1
            assert i < 256
            return stats[:, i:i + 1], rsc[:, i:i + 1], "st%d" % i

        def rearr_cp(v):
            return v.rearrange("(c p) -> p c", p=128)

        P.dma("sp", lambda e: e.dma_start(out=arena[:, 0:384], in_=consts_d), writes=["cF"])
        P.dma("sp", lambda e: e.dma_start(out=obias, in_=obias_d), writes=["obias"])
        P.dma("sp", lambda e: e.dma_start(out=ga2, in_=rearr_cp(g_attn), allow_slow_non_contiguous=True), writes=["ga2"])
        P.dma("sp", lambda e: e.dma_start(out=gf2, in_=rearr_cp(g_ffn), allow_slow_non_contiguous=True), writes=["gf2"])
        for k in range(3):
            P.dma("sp", lambda e, k=k: e.dma_start(out=cw[k], in_=rearr_cp(conv_w[k]), allow_slow_non_contiguous=True), writes=["convw%d" % k])
        P.dma("sp", lambda e: e.dma_start(out=cog, in_=rearr_cp(conv_out_g), allow_slow_non_contiguous=True), writes=["cog"])
        col = lambda v: v.rearrange("(p o) -> p o", o=1)
        P.dma("sp", lambda e: e.dma_start(out=gq2[0:64, :], in_=col(q_norm_g), allow_slow_non_contiguous=True), writes=["gq2a"])
        P.dma("sp", lambda e: e.dma_start(out=gq2[64:128, :], in_=col(q_norm_g), allow_slow_non_contiguous=True), writes=["gq2b"])
        P.dma("sp", lambda e: e.dma_start(out=gk2[0:64, :], in_=col(k_norm_g), allow_slow_non_contiguous=True), writes=["gk2a"])
        P.dma("sp", lambda e: e.dma_start(out=gk2[64:128, :], in_=col(k_norm_g), allow_slow_non_contiguous=True), writes=["gk2b"])
        P.dma("sp", lambda e: e.dma_start(out=sublnc, in_=col(subln_g), allow_slow_non_contiguous=True), writes=["sublnc"])
        for a, v in enumerate([lq1, lk1, lq2, lk2]):
            P.dma("sp", lambda e, a=a, v=v: e.dma_start(out=lamb[:, a, :], in_=v.partition_broadcast(128)), writes=["lamb%d" % a])
        P.op("pool", lambda e: e.memset(stats, 0.0), writes=["stats"])
        P.op("pool", lambda e: e.tensor_copy(out=identB, in_=identF), reads=["cF"], writes=["identB"])
        P.op("pool", lambda e: e.tensor_copy(out=triB, in_=triF), reads=["cF"], writes=["triB"])
        P.op("pool", lambda e: e.tensor_copy(out=b64B, in_=b64F), reads=["cF"], writes=["b64B"])
        P.op("dve", lambda e: e.tensor_scalar(out=gq2, in0=gq2, scalar1=0.125, scalar2=None, op0=ALU.mult), reads=["gq2a", "gq2b"], writes=["gq2"])
        P.op("dve", lambda e: e.tensor_copy(out=gk2, in_=gk2), reads=["gk2a", "gk2b"], writes=["gk2"])
        P.op("dve", lambda e: e.tensor_copy(out=wosc[:, 0:4], in_=cog), reads=["cog"], writes=["wosc_a"])
        for c in range(4, 8):
            P.op("dve", lambda e, c=c: e.tensor_scalar(out=wosc[:, c:c + 1], in0=sublnc, scalar1=0.8, scalar2=None, op0=ALU.mult), reads=["sublnc"], writes=["wosc%d" % c])
        P.op("dve", lambda e: e.tensor_tensor(out=lamb[:, 0, :], in0=lamb[:, 0, :], in1=lamb[:, 1, :], op=ALU.mult), reads=["lamb0", "lamb1"], writes=["lp0"])
        P.op("dve", lambda e: e.tensor_tensor(out=lamb[:, 2, :], in0=lamb[:, 2, :], in1=lamb[:, 3, :], op=ALU.mult), reads=["lamb2", "lamb3"], writes=["lp1"])
        P.op("dve", lambda e: e.reduce_sum(out=lamt[:, 0:1], in_=lamb[:, 0, :], axis=AX.X), reads=["lp0"], writes=["ls0"])
        P.op("dve", lambda e: e.reduce_sum(out=lamt[:, 1:2], in_=lamb[:, 2, :], axis=AX.X), reads=["lp1"], writes=["ls1"])
        P.op("act", lambda e: e.activation(out=lamt[:, 2:4], in_=lamt[:, 0:2], func=AF.Exp), reads=["ls0", "ls1"], writes=["le"])
        P.op("dve", lambda e: e.tensor_tensor(out=lamt[:, 4:5], in0=lamt[:, 3:4], in1=lamt[:, 2:3], op=ALU.subtract), reads=["le"], writes=["ld"])
        P.op("dve", lambda e: e.tensor_scalar(out=neglam, in0=lamt[:, 4:5], scalar1=-0.2, scalar2=None, op0=ALU.add), reads=["ld"], writes=["neglam"])

        R_KT = 3500
        R_VA = R_KT + 8192
        R_A = R_VA + 8256
        kT = arena[:, R_KT:R_KT + 8192].bitcast(BF16).rearrange("p (h t) -> p h t", h=4)
        VA = arena[:, R_VA:R_VA + 8256].bitcast(BF16).rearrange("p (s h v) -> p s h v", s=32, h=4)
        VA2 = arena[:, R_VA:R_VA + 8256].bitcast(BF16).rearrange("p (s v) -> p s v", v=129)
        P.op("pool", lambda e: e.memset(VA2[:, :, 128:129], 1.0), writes=["VAones"])

        w_in_v = w_in.rearrange("(c p) n -> p c n", p=128)

        def rmsnorm_A(xs_ap, xskey, npart, xn_ap, xnkey, junk_ap):
            ss, rs, sk = newstat()
            P.op("act", lambda e: e.activation(out=junk_ap[0:npart, :], in_=xs_ap[0:npart, :], func=AF.Square, accum_out=ss[0:npart, :]),
                 reads=[xskey, "stats"], writes=[sk, "junk"])
            P.op("act", lambda e: e.activation(out=rs[0:npart, :], in_=ss[0:npart, :], func=AF.Sqrt, scale=1.0 / 1024, bias=EPS), reads=[sk], writes=[sk + "r"])
            P.op("dve", lambda e: e.reciprocal(out=rs[0:npart, :], in_=rs[0:npart, :]), reads=[sk + "r"], writes=[sk + "r"])
            P.op("dve", lambda e: e.tensor_scalar(out=xn_ap[0:npart, :], in0=xs_ap[0:npart, :], scalar1=rs[0:npart, :], scalar2=None, op0=ALU.mult),
                 reads=[xskey, sk + "r"], writes=[xnkey])

        def rmsnorm_B(npart, xn_ap, xnkey, dsts, pst, pstkey):
            P.op("pe", [lambda e, c=c: e.transpose(out=pst[:, c, 0:npart], in_=xn_ap[0:npart, c * 128:(c + 1) * 128], identity=identB[0:npart, 0:npart]) for c in range(8)],
                 reads=[xnkey, "identB"], writes=[pstkey])
            for (dap, dkey, deng) in dsts:
                P.op("act", lambda e, dap=dap: e.copy(out=dap, in_=pst[:, :, 0:npart]), reads=[pstkey], writes=[dkey])

        def norm_feat(psrc, pskey, gcol, gkey, dst, dkey, sqb, sqkey, pS, pSkey, srt, srtkey, src_is_psum=True):
            P.op("act", lambda e: e.activation(out=sqb, in_=psrc, func=AF.Square), reads=[pskey], writes=[sqkey])
            P.op("pe", lambda e: e.matmul(pS, lhsT=b64B, rhs=sqb, start=True, stop=True), reads=[sqkey, "b64B"], writes=[pSkey])
            P.op("act", lambda e: e.activation(out=srt, in_=pS, func=AF.Sqrt, scale=1.0 / 64, bias=EPS), reads=[pSkey], writes=[srtkey])
            P.op("dve", lambda e: e.reciprocal(out=srt, in_=srt), reads=[srtkey], writes=[srtkey])
            if gcol is not None:
                P.op("dve", lambda e: e.scalar_tensor_tensor(out=dst, in0=psrc, scalar=gcol, in1=srt, op0=ALU.mult, op1=ALU.mult),
                     reads=[pskey, srtkey, gkey], writes=[dkey])
            else:
                P.op("dve", lambda e: e.tensor_tensor(out=dst, in0=psrc, in1=srt, op=ALU.mult), reads=[pskey, srtkey], writes=[dkey])

        if limit == 0:
            finish_early()
            return nc
        B1 = Bump(R_A, NCOL)
        hnTo = B1.bf(8 * 2080).rearrange("p (c t) -> p c t", c=8)
        wk_bf = B1.bf(8 * 512).rearrange("p (c n) -> p c n", c=8)
        wv_bf = B1.bf(8 * 512).rearrange("p (c n) -> p c n", c=8)
        wst = B1.f32(4096).rearrange("p (c n) -> p c n", c=8)
        hng = [B1.bf(8 * 512).rearrange("p (c t) -> p c t", c=8) for _ in range(2)]
        xs = [B1.f32(1024) for _ in range(2)]
        xn = [B1.bf(1024) for _ in range(2)]
        junk = B1.bf(1024)
        sqb = [B1.bf(512) for _ in range(2)]
        srt = [B1.f32(512) for _ in range(2)]
        pstT = [bank(k).bitcast(BF16).rearrange("p (c t) -> p c t", c=8) for k in (0, 1)]
        psK = [bank(2), bank(3)]
        psS = bank(4)
        psV = [bank(5), bank(6)]

        for wi, (wbf, c0) in enumerate([(wk_bf, 2048), (wv_bf, 2560)]):
            P.dma("sp", lambda e, c0=c0: e.dma_start(out=wst, in_=w_in_v[:, :, c0:c0 + 512]), writes=["wst"])
            for c in range(8):
                P.op("dve", lambda e, c=c, wbf=wbf: e.tensor_scalar(out=wbf[:, c, :], in0=wst[:, c, :], scalar1=ga2[:, c:c + 1], scalar2=None, op0=ALU.mult),
                     reads=["wst", "ga2"], writes=["wkv%d_%d" % (wi, c)])
        wk_keys = ["wkv0_%d" % c for c in range(8)]
        wv_keys = ["wkv1_%d" % c for c in range(8)]

        CUT = int(os.environ.get("KCUT", "0"))
        if CUT == 1:
            finish_early(); return nc
        tiles = [("h", 0, 0)] + [("t", tg, r) for tg in range(8) for r in range(4)]

        def stageA(t):
            kind, tg, r = tiles[t]
            b = t % 2
            if kind == "h":
                P.dma("sp", lambda e: e.dma_start(out=xs[b][0:32, :], in_=xh), writes=["xs%d" % b])
                rmsnorm_A(xs[b], "xs%d" % b, 32, xn[b], "xn%d" % b, junk)
            else:
                kt = 4 * tg + r
                P.dma("sp", lambda e, kt=kt, b=b: e.dma_start(out=xs[b], in_=xk[kt * 128:(kt + 1) * 128, :]), writes=["xs%d" % b])
                rmsnorm_A(xs[b], "xs%d" % b, 128, xn[b], "xn%d" % b, junk)

        def stageB(t):
            kind, tg, r = tiles[t]
            b = t % 2
            if kind == "h":
                rmsnorm_B(32, xn[b], "xn%d" % b, [(hnTo[:, :, 2048:2080], "hnTo_h", "act")], pstT[b], "pstT%d" % b)
                return
            g = hng[tg % 2]
            gk = "hng%d" % (tg % 2)
            kt = 4 * tg + r
            dsts = [(g[:, :, r * 128:(r + 1) * 128], gk + "_%d" % r, "act")]
            if r % 2 == 0:
                i = kt // 2
                dsts.append((hnTo[:, :, i * 128:(i + 1) * 128], "hnTo_%d" % i, "act"))
            rmsnorm_B(128, xn[b], "xn%d" % b, dsts, pstT[b], "pstT%d" % b)

        def kv_group(tg):
            g = hng[tg % 2]
            gk = "hng%d" % (tg % 2)
            gkeys = [gk + "_%d" % r for r in range(4)]
            for h in range(4):
                pb = (tg * 4 + h) % 2
                P.op("pe", [lambda e, c=c, h=h, pb=pb, g=g: e.matmul(psK[pb], lhsT=wk_bf[:, c, h * 128:(h + 1) * 128], rhs=g[:, c, :], start=(c == 0), stop=(c == 7)) for c in range(8)],
                     reads=gkeys + wk_keys, writes=["psK%d" % pb])
                norm_feat(psK[pb], "psK%d" % pb, gk2, "gk2", kT[:, h, tg * 512:(tg + 1) * 512], "kT", sqb[pb], "sqb%d" % pb, psS, "psS", srt[pb], "srt%d" % pb)
            for r in range(4):
                kt = 4 * tg + r
                pb = r % 2
                P.op("pe", [lambda e, c=c, r=r, pb=pb, g=g: e.matmul(psV[pb], lhsT=g[:, c, r * 128:(r + 1) * 128], rhs=wv_bf[:, c, :], start=(c == 0), stop=(c == 7)) for c in range(8)],
                     reads=[gk + "_%d" % r] + wv_keys, writes=["psV%d" % pb])
                P.op("act", lambda e, kt=kt, pb=pb: e.copy(out=VA[:, kt, :, 0:128], in_=psV[pb].rearrange("p (h v) -> p h v", h=4)), reads=["psV%d" % pb], writes=["VA"])

        stageA(0)
        for t in range(len(tiles)):
            if t + 1 < len(tiles):
                stageA(t + 1)
            stageB(t)
            kind, tg, r = tiles[t]
            if kind == "t" and r == 0 and tg > 0:
                kv_group(tg - 1)
        kv_group(7)
        P.barrier()

        if limit == 1:
            finish_early()
            return nc
        R_B = R_A + 8320
        B2 = Bump(R_B, NCOL)
        qT = B2.bf(4 * 2048).rearrange("p (h t) -> p h t", h=4)
        mixT = B2.bf(8 * 2048).rearrange("p (c t) -> p c t", c=8)
        R_C = B2.p
        _w = B2.f32(3072).rearrange("p (j c n) -> p j c n", j=3, c=8)
        wstg = [_w, _w]
        wblk = [B2.bf(3072).rearrange("p (j c n) -> p j c n", j=3, c=8) for _ in range(2)]
        sqb2 = [B2.bf(512) for _ in range(2)]
        srt2 = [B2.f32(512) for _ in range(2)]
        csb = [B2.f32(512) for _ in range(2)]
        ub = [B2.f32(520).rearrange("p (b t) -> p b t", b=4) for _ in range(2)]
        yb = [B2.f32(512) for _ in range(2)]
        uh = B2.f32(128).rearrange("p (c t) -> p c t", c=4)
        hcs = B2.f32(32)
        psA = [bank(0), bank(1)]
        psB = [bank(2), bank(3)]
        psC = [bank(4), bank(5)]
        psS2 = bank(6)
        psH = bank(7)

        jobs = [("q", h, [1536 + h * 128]) for h in range(4)] + [("c", cc, [cc * 128, 512 + cc * 128, 1024 + cc * 128]) for cc in range(4)]
        wkeys = {}

        def load_job(ji):
            kind, idx, cols = jobs[ji]
            b = ji % 2
            keys = []
            for j, c0 in enumerate(cols):
                P.dma("sp", lambda e, b=b, j=j, c0=c0: e.dma_start(out=wstg[b][:, j], in_=w_in_v[:, :, c0:c0 + 128]), writes=["wstg_%d" % j])
                P.op("dve", [lambda e, b=b, j=j, c=c: e.tensor_scalar(out=wblk[b][:, j, c, :], in0=wstg[b][:, j, c, :], scalar1=ga2[:, c:c + 1], scalar2=None, op0=ALU.mult) for c in range(8)],
                     reads=["wstg_%d" % j, "ga2"], writes=["wblk%d_%d" % (b, j)])
                keys.append("wblk%d_%d" % (b, j))
            wkeys[ji] = keys

        load_job(0)
        cnt2 = 0
        for ji, (kind, idx, cols) in enumerate(jobs):
            if ji + 1 < len(jobs):
                load_job(ji + 1)
            b = ji % 2
            wb = wblk[b]
            if kind == "q":
                h = idx
                for tg in range(4):
                    pb = cnt2 % 2
                    cnt2 += 1
                    P.op("pe", [lambda e, c=c, pb=pb, tg=tg, wb=wb: e.matmul(psA[pb], lhsT=wb[:, 0, c, :], rhs=hnTo[:, c, tg * 512:(tg + 1) * 512], start=(c == 0), stop=(c == 7)) for c in range(8)],
                         reads=wkeys[ji], writes=["psA%d" % pb])
                    norm_feat(psA[pb], "psA%d" % pb, gq2, "gq2", qT[:, h, tg * 512:(tg + 1) * 512], "qT", sqb2[pb], "sqb2%d" % pb, psS2, "psS2", srt2[pb], "srt2%d" % pb)
            else:
                cc = idx
                P.op("pe", [lambda e, c=c, wb=wb: e.matmul(psH[:, 0:32], lhsT=wb[:, 0, c, :], rhs=hnTo[:, c, 2048:2080], start=(c == 0), stop=(c == 7)) for c in range(8)],
                     reads=wkeys[ji], writes=["psHx"])
                P.op("pe", [lambda e, c=c, wb=wb: e.matmul(psH[:, 64:96], lhsT=wb[:, 2, c, :], rhs=hnTo[:, c, 2048:2080], start=(c == 0), stop=(c == 7)) for c in range(8)],
                     reads=wkeys[ji], writes=["psHc"])
                P.op("act", lambda e: e.copy(out=hcs, in_=psH[:, 64:96]), reads=["psHc"], writes=["hcs"])
                P.op("dve", lambda e, cc=cc: e.tensor_tensor(out=uh[:, cc, :], in0=psH[:, 0:32], in1=hcs, op=ALU.mult), reads=["psHx", "hcs"], writes=["uh%d" % cc])
                for tg in range(4):
                    pb = cnt2 % 2
                    cnt2 += 1
                    tsl = slice(tg * 512, (tg + 1) * 512)
                    for j, (pp, nm) in enumerate([(psA, "psA"), (psB, "psB"), (psC, "psC")]):
                        P.op("pe", [lambda e, c=c, pb=pb, j=j, pp=pp, wb=wb, tsl=tsl: e.matmul(pp[pb], lhsT=wb[:, j, c, :], rhs=hnTo[:, c, tsl], start=(c == 0), stop=(c == 7)) for c in range(8)],
                             reads=wkeys[ji], writes=["%s%d" % (nm, pb)])
                    u = ub[pb]
                    y = yb[pb]
                    y3 = y.rearrange("p (b t) -> p b t", b=4)
                    P.op("act", lambda e, pb=pb: e.copy(out=csb[pb], in_=psC[pb]), reads=["psC%d" % pb], writes=["csb%d" % pb])
                    P.op("dve", lambda e, pb=pb, u=u: e.tensor_tensor(out=u[:, :, 2:130], in0=psA[pb].rearrange("p (b t) -> p b t", b=4), in1=csb[pb].rearrange("p (b t) -> p b t", b=4), op=ALU.mult),
                         reads=["psA%d" % pb, "csb%d" % pb], writes=["u%d" % pb])
                    P.op("pool", lambda e, u=u, cc=cc, tg=tg: e.tensor_copy(out=u[:, :, 0:2], in_=uh[:, cc, tg * 8:(tg + 1) * 8].rearrange("p (b t) -> p b t", b=4)),
                         reads=["uh%d" % cc], writes=["uhalo%d" % pb])
                    P.op("dve", lambda e, u=u, y3=y3, cc=cc: e.tensor_scalar(out=y3, in0=u[:, :, 0:128], scalar1=cw[0][:, cc:cc + 1], scalar2=None, op0=ALU.mult),
                         reads=["u%d" % pb, "uhalo%d" % pb, "convw0"], writes=["y%d" % pb])
                    for k in (1, 2):
                        P.op("dve", lambda e, u=u, y3=y3, cc=cc, k=k: e.scalar_tensor_tensor(out=y3, in0=u[:, :, k:k + 128], scalar=cw[k][:, cc:cc + 1], in1=y3, op0=ALU.mult, op1=ALU.add),
                             reads=["u%d" % pb, "uhalo%d" % pb, "y%d" % pb, "convw%d" % k], writes=["y%d" % pb])
                    P.op("dve", lambda e, y=y, pb=pb: e.tensor_tensor(out=y, in0=y, in1=psB[pb], op=ALU.mult), reads=["y%d" % pb, "psB%d" % pb], writes=["y%d" % pb])
                    norm_feat(y, "y%d" % pb, None, None, mixT[:, cc, tsl], "mixT", sqb2[pb], "sqb2%d" % pb, psS2, "psS2", srt2[pb], "srt2%d" % pb)
        P.barrier()

        if limit == 2:
            finish_early()
            return nc
        B3 = Bump(R_C, NCOL)
        PT = [B3.bf(512) for _ in range(4)]
        r0b = B3.f32(512)
        r1b = B3.f32(512)
        of_ = B3.f32(512)
        tf_ = B3.f32(512)
        sq3 = B3.bf(512)
        srt3 = B3.f32(512)
        onesB = B3.bf(128)
        P.op("pool", lambda e: e.memset(onesB, 1.0), writes=["onesB"])
        R_D = B3.p
        B3b = Bump(R_B - 8192, R_B)
        wo_bf = B3b.bf(8 * 1024).rearrange("p (c n) -> p c n", c=8)
        wost = B3b.f32(4096).rearrange("p (c n) -> p c n", c=8)
        w_out_v = w_out.rearrange("(c p) n -> p c n", p=128)
        for half in range(2 if os.environ.get('KV') != 'C' else 0):
            P.dma("sp", lambda e, half=half: e.dma_start(out=wost, in_=w_out_v[:, :, half * 512:(half + 1) * 512]), writes=["wost"])
            for c in range(8):
                P.op("dve", lambda e, c=c, half=half: e.tensor_scalar(out=wo_bf[:, c, half * 512:(half + 1) * 512], in0=wost[:, c, :], scalar1=wosc[:, c:c + 1], scalar2=None, op0=ALU.mult),
                     reads=["wost", "wosc_a"] + ["wosc%d" % k for k in range(4, 8)], writes=["wo_bf%d_%d" % (half, c)])
        wo_keys = ["wo_bf%d_%d" % (hf, c) for hf in range(2) for c in range(8)]

        psST = [[bank(4), bank(5)], [bank(6), bank(7)]]
        psOT = [bank(0), bank(1)]
        psSS = [bank(2), bank(3)]
        psN = bank(7)
        psNkey = "psST1_1"

        steps = []
        for G in range(4):
            for h in range(4):
                for j in range(8 * G + 8):
                    steps.append((G, h, j))
        nsteps = len(steps)

        def geom(G, j):
            pj = j // 2
            i0 = max(pj, 4 * G)
            nq = (4 * G + 4 - i0) * 128
            return pj, i0, nq

        def emit_ST(si):
            G, h, j = steps[si]
            pj, i0, nq = geom(G, j)
            q0 = i0 * 128
            sb = si % 2
            P.op("pe", [lambda e, c=c: e.matmul(psST[sb][c][:, 0:nq], lhsT=kT[c * 64:(c + 1) * 64, h, j * 128:(j + 1) * 128], rhs=qT[c * 64:(c + 1) * 64, h, q0:q0 + nq], start=True, stop=True) for c in range(2)],
                 reads=["kT", "qT"], writes=["psST%d_0" % sb, "psST%d_1" % sb])

        def emit_exp_pv(si):
            G, h, j = steps[si]
            pj, i0, nq = geom(G, j)
            qoff = 512 - nq
            sb = si % 2
            special = pj >= 4 * G
            allk = []
            pts = []
            for c in range(2):
                pt = PT[sb * 2 + c]
                ptk = "PT%d" % (sb * 2 + c)
                src = psST[sb][c]
                sk_ = "psST%d_%d" % (sb, c)
                if special and j % 2 == 1:
                    P.op("act", lambda e, pt=pt, src=src: e.activation(out=pt[:, 0:128], in_=src[:, 0:128], func=AF.Exp, bias=obias[:, pj % 2:pj % 2 + 1], scale=1.0),
                         reads=[sk_, "obias"], writes=[ptk])
                    if nq > 128:
                        P.op("act", lambda e, pt=pt, src=src: e.activation(out=pt[:, 128:nq], in_=src[:, 128:nq], func=AF.Exp), reads=[sk_], writes=[ptk + "r"])
                        allk += [ptk, ptk + "r"]
                    else:
                        allk += [ptk]
                else:
                    P.op("act", lambda e, pt=pt, src=src: e.activation(out=pt[:, 0:nq], in_=src[:, 0:nq], func=AF.Exp), reads=[sk_], writes=[ptk, ptk + "r"])
                    allk += [ptk, ptk + "r"]
                    if special:
                        P.op("pool", lambda e, pt=pt: e.tensor_tensor(out=pt[:, 0:128], in0=pt[:, 0:128], in1=triB, op=ALU.mult), reads=[ptk, "triB"], writes=[ptk])
                pts.append(pt)
            last = (j == 8 * G + 7)
            fns = []
            for c in range(2):
                fns.append(lambda e, c=c: e.matmul(psOT[c][:, qoff:512], lhsT=VA[:, j, h, 0:128], rhs=pts[c][:, 0:nq], start=(j == 0), stop=last))
                fns.append(lambda e, c=c: e.matmul(psSS[c][:, qoff:512], lhsT=onesB, rhs=pts[c][:, 0:nq], start=(j == 0), stop=last))
            P.op("pe", fns, reads=allk + ["VA", "onesB"], writes=["psOT0", "psSS0", "psOT1", "psSS1"])

        def emit_final_a(G, h):
            P.op("dve", lambda e: e.reciprocal(out=r0b, in_=psSS[0]), reads=["psSS0"], writes=["r0b"])
            P.op("dve", lambda e: e.reciprocal(out=r1b, in_=psSS[1]), reads=["psSS1"], writes=["r1b"])
            P.op("dve", lambda e: e.tensor_tensor(out=of_, in0=psOT[0], in1=r0b, op=ALU.mult), reads=["psOT0", "r0b"], writes=["of"])
            P.op("dve", lambda e: e.tensor_tensor(out=tf_, in0=psOT[1], in1=r1b, op=ALU.mult), reads=["psOT1", "r1b"], writes=["tf"])
            P.op("dve", lambda e: e.scalar_tensor_tensor(out=of_, in0=tf_, scalar=neglam, in1=of_, op0=ALU.mult, op1=ALU.add), reads=["tf", "of", "neglam"], writes=["of"])

        def emit_final_b(G, h):
            P.op("act", lambda e: e.activation(out=sq3, in_=of_, func=AF.Square), reads=["of"], writes=["sq3"])
            P.op("pe", lambda e: e.matmul(psN, lhsT=onesB, rhs=sq3, start=True, stop=True), reads=["sq3", "onesB"], writes=[psNkey])
            P.op("act", lambda e: e.activation(out=srt3, in_=psN, func=AF.Sqrt, scale=1.0 / 128, bias=EPS), reads=[psNkey], writes=["srt3"])
            P.op("dve", lambda e: e.reciprocal(out=srt3, in_=srt3), reads=["srt3"], writes=["srt3"])
            P.op("dve", lambda e: e.tensor_tensor(out=mixT[:, 4 + h, G * 512:(G + 1) * 512], in0=of_, in1=srt3, op=ALU.mult), reads=["of", "srt3"], writes=["mixT"])

        pending = []
        emit_ST(0)
        for si in range(nsteps):
            if si + 1 < nsteps:
                emit_ST(si + 1)
            emit_exp_pv(si)
            G, h, j = steps[si]
            if pending and si >= pending[0][0]:
                _, pg, ph = pending.pop(0)
                emit_final_b(pg, ph)
            if j == 8 * G + 7:
                emit_final_a(G, h)
                pending.append((si + 4, G, h))
        for _, pg, ph in pending:
            emit_final_b(pg, ph)
        P.barrier()

        if limit == 3:
            finish_early()
            return nc
        B4 = Bump(R_KT, R_A)
        hn2T = B4.bf(8 * 2048).rearrange("p (c t) -> p c t", c=8)
        combT = [B4.bf(2048) for _ in range(2)]
        R_E = B4.p
        B4 = Bump(R_D, NCOL)
        xs4 = [B4.f32(1024) for _ in range(2)]
        ht = [B4.f32(1024) for _ in range(2)]
        hn2 = [B4.bf(1024) for _ in range(2)]
        junk4 = B4.bf(1024)
        wrst = B4.f32(8 * 36).rearrange("p (c n) -> p c n", c=8)
        wr_bf = B4.bf(8 * 36).rearrange("p (c n) -> p c n", c=8)
        Lb = [B4.f32(36) for _ in range(2)]
        rt = [B4.f32(160) for _ in range(2)]
        chl_all = B4.bf(16 * 64).rearrange("p (i k) -> p i k", i=16)
        psO = [(bank(0), bank(1)), (bank(4), bank(5))]
        psT4 = [bank(2).bitcast(BF16).rearrange("p (c t) -> p c t", c=8) for k in (0, 1)]
        psL = [bank(3)[:, 0:36], bank(3)[:, 0:36]]
        psCm = [bank(6).bitcast(BF16)[:, 0:256], bank(7).bitcast(BF16)[:, 0:256]]
        psMT = bank(5)[:, 0:256].bitcast(BF16)

        P.dma("sp", lambda e: e.dma_start(out=wrst[:, :, 0:4], in_=wrg.rearrange("(c p) n -> p c n", p=128), allow_slow_non_contiguous=True), writes=["wrst_a"])
        P.dma("sp", lambda e: e.dma_start(out=wrst[:, :, 4:36], in_=wre.rearrange("(c p) n -> p c n", p=128), allow_slow_non_contiguous=True), writes=["wrst_b"])
        P.op("dve", [lambda e, c=c: e.tensor_scalar(out=wr_bf[:, c, :], in0=wrst[:, c, :], scalar1=gf2[:, c:c + 1], scalar2=None, op0=ALU.mult) for c in range(8)],
             reads=["wrst_a", "wrst_b", "gf2"], writes=["wr_bf"])
        for q4 in range(4):
            P.dma("sp", lambda e, q4=q4: e.dma_start(out=xs4[0][0:32, :], in_=sel_d[:, q4 * 1024:(q4 + 1) * 1024]), writes=["xs4_0"])
            P.op("pool", lambda e, q4=q4: e.tensor_copy(out=selB[0:32, q4 * 1024:(q4 + 1) * 1024], in_=xs4[0][0:32, :]), reads=["xs4_0"], writes=["selB"])

        BIG = 10000.0
        def front(i):
            b = i % 2
            P.dma("sp", lambda e, i=i, b=b: e.dma_start(out=xs4[b], in_=xk[(2 * i) * 128:(2 * i + 1) * 128, :]), writes=["xs4_%d" % b])
            for half in range(2):
                P.op("pe", [lambda e, c=c, half=half, i=i, b=b: e.matmul(psO[b][half], lhsT=mixT[:, c, i * 128:(i + 1) * 128], rhs=wo_bf[:, c, half * 512:(half + 1) * 512], start=(c == 0), stop=(c == 7)) for c in range(8)],
                     reads=["mixT"] + wo_keys, writes=["psO%d_%d" % (b, half)])
                P.op("dve", lambda e, half=half, b=b: e.tensor_tensor(out=ht[b][:, half * 512:(half + 1) * 512], in0=psO[b][half], in1=xs4[b][:, half * 512:(half + 1) * 512], op=ALU.add),
                     reads=["psO%d_%d" % (b, half), "xs4_%d" % b], writes=["ht%d_%d" % (b, half)])
            htk = ["ht%d_0" % b, "ht%d_1" % b]
            P.dma("pool", lambda e, i=i, b=b: e.dma_start(out=hscr[i * 128:(i + 1) * 128, :], in_=ht[b]), reads=htk, writes=["hscr"])
            ss, rs, sk = newstat()
            P.op("act", lambda e, b=b, ss=ss: e.activation(out=junk4, in_=ht[b], func=AF.Square, accum_out=ss), reads=htk + ["stats"], writes=[sk, "junk4"])
            P.op("act", lambda e, ss=ss, rs=rs: e.activation(out=rs, in_=ss, func=AF.Sqrt, scale=1.0 / 1024, bias=EPS), reads=[sk], writes=[sk + "r"])
            P.op("dve", lambda e, rs=rs: e.reciprocal(out=rs, in_=rs), reads=[sk + "r"], writes=[sk + "r"])
            P.op("dve", lambda e, b=b, rs=rs: e.tensor_scalar(out=hn2[b], in0=ht[b], scalar1=rs, scalar2=None, op0=ALU.mult), reads=htk + [sk + "r"], writes=["hn2_%d" % b])

        def back(i):
            b = i % 2
            P.op("pe", [lambda e, c=c, b=b: e.transpose(out=psT4[b][:, c, :], in_=hn2[b][:, c * 128:(c + 1) * 128], identity=identB) for c in range(8)],
                 reads=["hn2_%d" % b, "identB"], writes=["psT4"])
            P.op("act", lambda e, b=b, i=i: e.copy(out=hn2T[:, :, i * 128:(i + 1) * 128], in_=psT4[b]), reads=["psT4"], writes=["hn2T_%d" % i])
            P.op("pe", [lambda e, c=c, b=b, i=i: e.matmul(psL[b], lhsT=hn2T[:, c, i * 128:(i + 1) * 128], rhs=wr_bf[:, c, :], start=(c == 0), stop=(c == 7)) for c in range(8)],
                 reads=["hn2T_%d" % i, "wr_bf"], writes=["psL"])
            L = Lb[b]
            T = rt[b]
            lk = "L%d" % b
            tk = "rt%d_" % b
            P.op("act", lambda e, L=L, b=b: e.copy(out=L, in_=psL[b]), reads=["psL"], writes=[lk])
            mg, nmg, eg, sg, gg, oh, pen = T[:, 0:1], T[:, 1:2], T[:, 2:6], None, T[:, 7:8], T[:, 8:12], T[:, 12:16]
            LM = T[:, 16:48]
            m1, mk1, LM2, m2, mk2 = T[:, 48:49], T[:, 49:81], T[:, 81:113], T[:, 113:114], T[:, 114:146]
            dd, ed, den, w1, w2 = T[:, 146:147], T[:, 147:148], T[:, 148:149], T[:, 149:150], T[:, 150:151]
            comb = T[:, 16:48]
            comb = T[:, 81:113]
            ssg, _, skg = newstat()
            P.op("dve", lambda e, L=L, mg=mg: e.reduce_max(out=mg, in_=L[:, 0:4], axis=AX.X), reads=[lk], writes=[tk + "mg"])
            P.op("dve", lambda e, mg=mg, nmg=nmg: e.tensor_scalar(out=nmg, in0=mg, scalar1=-1.0, scalar2=None, op0=ALU.mult), reads=[tk + "mg"], writes=[tk + "nmg"])
            P.op("act", lambda e, L=L, eg=eg, nmg=nmg, ssg=ssg: e.activation(out=eg, in_=L[:, 0:4], func=AF.Exp, bias=nmg, scale=1.0, accum_out=ssg), reads=[lk, tk + "nmg", "stats"], writes=[tk + "eg", skg])
            P.op("dve", lambda e, gg=gg, ssg=ssg: e.reciprocal(out=gg, in_=ssg), reads=[skg], writes=[tk + "gg"])
            P.op("dve", lambda e, L=L, oh=oh, mg=mg: e.tensor_scalar(out=oh, in0=L[:, 0:4], scalar1=mg, scalar2=None, op0=ALU.is_ge), reads=[lk, tk + "mg"], writes=[tk + "oh"])
            P.op("dve", lambda e, oh=oh, pen=pen: e.tensor_scalar(out=pen, in0=oh, scalar1=BIG, scalar2=-BIG, op0=ALU.mult, op1=ALU.add), reads=[tk + "oh"], writes=[tk + "pen"])
            for g in range(4):
                P.op("dve", lambda e, L=L, LM=LM, pen=pen, g=g: e.tensor_scalar(out=LM[:, g * 8:(g + 1) * 8], in0=L[:, 4 + g * 8:12 + g * 8], scalar1=pen[:, g:g + 1], scalar2=None, op0=ALU.add),
                     reads=[lk, tk + "pen"], writes=[tk + "LM"])
            P.op("dve", lambda e, LM=LM, m1=m1: e.reduce_max(out=m1, in_=LM, axis=AX.X), reads=[tk + "LM"], writes=[tk + "m1"])
            P.op("dve", lambda e, LM=LM, m1=m1, mk1=mk1: e.tensor_scalar(out=mk1, in0=LM, scalar1=m1, scalar2=None, op0=ALU.is_ge), reads=[tk + "LM", tk + "m1"], writes=[tk + "mk1"])
            P.op("dve", lambda e, LM=LM, mk1=mk1, LM2=LM2: e.scalar_tensor_tensor(out=LM2, in0=mk1, scalar=-BIG, in1=LM, op0=ALU.mult, op1=ALU.add), reads=[tk + "LM", tk + "mk1"], writes=[tk + "LM2"])
            P.op("dve", lambda e, LM2=LM2, m2=m2: e.reduce_max(out=m2, in_=LM2, axis=AX.X), reads=[tk + "LM2"], writes=[tk + "m2"])
            P.op("dve", lambda e, LM2=LM2, m2=m2, mk2=mk2: e.tensor_scalar(out=mk2, in0=LM2, scalar1=m2, scalar2=None, op0=ALU.is_ge), reads=[tk + "LM2", tk + "m2"], writes=[tk + "mk2"])
            P.op("dve", lambda e, dd=dd, m1=m1, m2=m2: e.tensor_tensor(out=dd, in0=m2, in1=m1, op=ALU.subtract), reads=[tk + "m1", tk + "m2"], writes=[tk + "dd"])
            P.op("act", lambda e, dd=dd, ed=ed: e.activation(out=ed, in_=dd, func=AF.Exp), reads=[tk + "dd"], writes=[tk + "ed"])
            P.op("dve", lambda e, ed=ed, den=den: e.tensor_scalar(out=den, in0=ed, scalar1=1.0, scalar2=None, op0=ALU.add), reads=[tk + "ed"], writes=[tk + "den"])
            P.op("dve", lambda e, den=den, w1=w1: e.reciprocal(out=w1, in_=den), reads=[tk + "den"], writes=[tk + "w1"])
            P.op("dve", lambda e, w1=w1, gg=gg: e.tensor_tensor(out=w1, in0=w1, in1=gg, op=ALU.mult), reads=[tk + "w1", tk + "gg"], writes=[tk + "w1"])
            P.op("dve", lambda e, w1=w1, w2=w2, ed=ed: e.tensor_tensor(out=w2, in0=w1, in1=ed, op=ALU.mult), reads=[tk + "w1", tk + "ed"], writes=[tk + "w2"])
            P.op("dve", lambda e, comb=comb, mk2=mk2, w2=w2: e.tensor_scalar(out=comb, in0=mk2, scalar1=w2, scalar2=None, op0=ALU.mult), reads=[tk + "mk2", tk + "w2", tk + "LM2"], writes=[tk + "LM2"])
            P.op("dve", lambda e, comb=comb, mk1=mk1, w1=w1: e.scalar_tensor_tensor(out=comb, in0=mk1, scalar=w1, in1=comb, op0=ALU.mult, op1=ALU.add), reads=[tk + "mk1", tk + "w1", tk + "LM2"], writes=[tk + "comb"])
            ch = chl_all[:, i, :]
            P.op("dve", lambda e, comb=comb, ch=ch: e.tensor_copy(out=ch[:, 0:32], in_=comb), reads=[tk + "comb"], writes=["chl%d_h" % i])
            P.op("dve", lambda e, comb=comb, ch=ch: e.tensor_tensor(out=ch[:, 32:64], in0=comb, in1=ch[:, 0:32], op=ALU.subtract), reads=[tk + "comb", "chl%d_h" % i], writes=["chl%d_l" % i])

        front(0)
        for i in range(16):
            if i + 1 < 16:
                front(i + 1)
            back(i)
        for i in range(16):
            b = i % 2
            ch = chl_all[:, i, :]
            P.op("pe", [lambda e, ch=ch, b=b: e.transpose(out=psCm[b][0:32, 0:128], in_=ch[:, 0:32], identity=identB),
                        lambda e, ch=ch, b=b: e.transpose(out=psCm[b][0:32, 128:256], in_=ch[:, 32:64], identity=identB)],
                 reads=["chl%d_h" % i, "chl%d_l" % i, "identB"], writes=["psCm%d" % b])
            P.op("act", lambda e, b=b, i=i: e.copy(out=combT[0][0:32, i * 128:(i + 1) * 128], in_=psCm[b][0:32, 0:128]), reads=["psCm%d" % b], writes=["combT0_%d" % (i // 4)])
            P.op("act", lambda e, b=b, i=i: e.copy(out=combT[1][0:32, i * 128:(i + 1) * 128], in_=psCm[b][0:32, 128:256]), reads=["psCm%d" % b], writes=["combT1_%d" % (i // 4)])
        P.barrier()

        if limit == 4:
            finish_early()
            return nc
        B5 = Bump(R_E, NCOL)
        yacc = B5.f32(8 * 2048).rearrange("p (c t) -> p c t", c=8)
        wgb = [B5.bf(8 * 512).rearrange("p (c n) -> p c n", c=8) for _ in range(2)]
        wub = [B5.bf(8 * 512).rearrange("p (c n) -> p c n", c=8) for _ in range(2)]
        wdb = [B5.bf(4 * 1024).rearrange("p (c n) -> p c n", c=4) for _ in range(2)]
        wst5 = [B5.f32(2048).rearrange("p (c n) -> p c n", c=4) for _ in range(2)]
        actT = [[B5.bf(512) for _ in range(4)] for _ in range(2)]
        sb5 = [B5.f32(512) for _ in range(2)]
        tb5 = [B5.f32(512) for _ in range(2)]
        cbs = [B5.f32(512) for _ in range(2)]
        psG = [bank(0), bank(1)]
        psU = [bank(2), bank(3)]
        psY = [bank(4), bank(5)]
        psCB = bank(6)

        piece = [0]

        def load_expert(ex, which):
            wb = ex % 2
            srcs = []
            gv = weg[ex].rearrange("(c p) n -> p c n", p=128)
            uv = weu[ex].rearrange("(c p) n -> p c n", p=128)
            dv = wed[ex].rearrange("(c p) n -> p c n", p=128)
            srcs.append((gv[:, 0:4, :], wgb[wb], 0, "g", True))
            srcs.append((gv[:, 4:8, :], wgb[wb], 4, "g", True))
            srcs.append((uv[:, 0:4, :], wub[wb], 0, "u", True))
            srcs.append((uv[:, 4:8, :], wub[wb], 4, "u", True))
            srcs.append((dv[:, :, 0:512], wdb[wb], 0, "d", False))
            srcs.append((dv[:, :, 512:1024], wdb[wb], 512, "d", False))
            for (src, dst, off, nm, scaled) in [srcs[k] for k in which]:
                sbi = piece[0] % 2
                piece[0] += 1
                P.dma("sp", lambda e, src=src, sbi=sbi: e.dma_start(out=wst5[sbi], in_=src), writes=["wst5_%d" % sbi])
                if scaled:
                    for c in range(4):
                        P.op("act", lambda e, c=c, dst=dst, off=off, sbi=sbi: e.mul(out=dst[:, off + c, :], in_=wst5[sbi][:, c, :], mul=gf2[:, off + c:off + c + 1]),
                             reads=["wst5_%d" % sbi, "gf2"], writes=["w%s%d_%d" % (nm, wb, off + c)])
                else:
                    P.op("dve", lambda e, dst=dst, off=off, sbi=sbi: e.tensor_copy(out=dst[:, :, off:off + 512], in_=wst5[sbi]),
                         reads=["wst5_%d" % sbi], writes=["wd%d_%d" % (wb, off)])

        def gu_keys(wb):
            return ["wg%d_%d" % (wb, c) for c in range(8)] + ["wu%d_%d" % (wb, c) for c in range(8)]

        def emit_GU(ex, tb, ab):
            wb = ex % 2
            tsl = slice(tb * 512, (tb + 1) * 512)
            cb = (ex * 4 + tb) % 2
            P.op("pe", [lambda e: e.matmul(psCB, lhsT=selB[0:32, ex * 128:(ex + 1) * 128], rhs=combT[0][0:32, tsl], start=True, stop=False),
                        lambda e: e.matmul(psCB, lhsT=selB[0:32, ex * 128:(ex + 1) * 128], rhs=combT[1][0:32, tsl], start=False, stop=True)],
                 reads=["selB", "combT0_%d" % tb, "combT1_%d" % tb], writes=["psCB"])
            P.op("act", lambda e: e.copy(out=cbs[cb], in_=psCB), reads=["psCB"], writes=["cbs%d" % cb])
            for fc in range(4):
                pb = fc % 2
                P.op("pe", [lambda e, c=c, fc=fc, pb=pb: e.matmul(psG[pb], lhsT=wgb[wb][:, c, fc * 128:(fc + 1) * 128], rhs=hn2T[:, c, tsl], start=(c == 0), stop=(c == 7)) for c in range(8)],
                     reads=["wg%d_%d" % (wb, c) for c in range(8)], writes=["psG%d" % pb])
                P.op("pe", [lambda e, c=c, fc=fc, pb=pb: e.matmul(psU[pb], lhsT=wub[wb][:, c, fc * 128:(fc + 1) * 128], rhs=hn2T[:, c, tsl], start=(c == 0), stop=(c == 7)) for c in range(8)],
                     reads=["wu%d_%d" % (wb, c) for c in range(8)], writes=["psU%d" % pb])
                P.op("act", lambda e, pb=pb: e.activation(out=sb5[pb], in_=psG[pb], func=AF.Silu), reads=["psG%d" % pb], writes=["sb5_%d" % pb])
                P.op("dve", lambda e, pb=pb: e.tensor_tensor(out=tb5[pb], in0=sb5[pb], in1=psU[pb], op=ALU.mult), reads=["sb5_%d" % pb, "psU%d" % pb], writes=["tb5_%d" % pb])
                P.op("pool", lambda e, pb=pb, fc=fc: e.tensor_tensor(out=actT[ab][fc], in0=tb5[pb], in1=cbs[cb], op=ALU.mult), reads=["tb5_%d" % pb, "cbs%d" % cb], writes=["actT%d_%d" % (ab, fc)])

        dcount = [0]

        def emit_DOWN(ex, tb, ab):
            wb = ex % 2
            tsl = slice(tb * 512, (tb + 1) * 512)
            for dc in range(8):
                pb = dcount[0] % 2
                dcount[0] += 1
                P.op("pe", [lambda e, fc=fc, pb=pb, dc=dc: e.matmul(psY[pb], lhsT=wdb[wb][:, fc, dc * 128:(dc + 1) * 128], rhs=actT[ab][fc], start=(fc == 0), stop=(fc == 3)) for fc in range(4)],
                     reads=["actT%d_%d" % (ab, fc) for fc in range(4)] + ["wd%d_0" % wb, "wd%d_512" % wb], writes=["psY%d" % pb])
                if ex == 0:
                    P.op("dve", lambda e, pb=pb, dc=dc: e.tensor_copy(out=yacc[:, dc, tsl], in_=psY[pb]), reads=["psY%d" % pb], writes=["yacc"])
                else:
                    P.op("dve", lambda e, pb=pb, dc=dc: e.tensor_tensor(out=yacc[:, dc, tsl], in0=yacc[:, dc, tsl], in1=psY[pb], op=ALU.add), reads=["psY%d" % pb, "yacc"], writes=["yacc"])

        load_expert(0, range(6))
        work = [(ex, tb) for ex in range(n_exp) for tb in range(4)]
        for wi, (ex, tb) in enumerate(work):
            emit_GU(ex, tb, wi % 2)
            if wi > 0:
                pex, ptb = work[wi - 1]
                emit_DOWN(pex, ptb, (wi - 1) % 2)
            if ex + 1 < n_exp and tb < 3:
                load_expert(ex + 1, [2 * tb, 2 * tb + 1])
        pex, ptb = work[-1]
        emit_DOWN(pex, ptb, (len(work) - 1) % 2)
        P.barrier()

        if limit == 5:
            finish_early()
            return nc
        B6 = Bump(R_E + 16384, NCOL)
        hl = [B6.f32(1024) for _ in range(2)]
        ob6 = [B6.f32(1024) for _ in range(2)]
        psF = [(bank(0), bank(1)), (bank(2), bank(3))]
        for i in range(16):
            b = i % 2
            P.dma("sp", lambda e, i=i, b=b: e.dma_start(out=hl[b], in_=hscr[i * 128:(i + 1) * 128, :]), reads=["hscr"], writes=["hl%d" % b])
            for half in range(2):
                P.op("pe", [lambda e, dcl=dcl, half=half, i=i, b=b: e.transpose(out=psF[b][half][:, dcl * 128:(dcl + 1) * 128], in_=yacc[:, half * 4 + dcl, i * 128:(i + 1) * 128], identity=identF) for dcl in range(4)],
                     reads=["yacc", "cF"], writes=["psF%d_%d" % (b, half)])
                P.op("dve", lambda e, half=half, b=b: e.tensor_tensor(out=ob6[b][:, half * 512:(half + 1) * 512], in0=psF[b][half], in1=hl[b][:, half * 512:(half + 1) * 512], op=ALU.add),
                     reads=["psF%d_%d" % (b, half), "hl%d" % b], writes=["ob6_%d_%d" % (b, half)])
            P.dma("pool", lambda e, i=i, b=b: e.dma_start(out=out[i * 128:(i + 1) * 128, :], in_=ob6[b]), reads=["ob6_%d_0" % b, "ob6_%d_1" % b], writes=["out"])
        P.final_wait("pool", ["out"])
        P.final_wait("sp", ["out"])
        P.emit(nc, st)
    return nc


def _own_block(i, half):
    return 2 * i + ((i + half) % 2)


def _host_consts():
    ident = np.eye(128, dtype=np.float32)
    k = np.arange(128)[:, None]
    q = np.arange(128)[None, :]
    tri = (k <= q).astype(np.float32)
    b64 = (k // 64 == q // 64).astype(np.float32)
    consts = np.concatenate([ident, tri, b64], axis=1)
    sel = np.zeros((32, 32, 128), np.float32)
    for e in range(32):
        sel[e, e, :] = 1.0
    return np.ascontiguousarray(consts), np.ascontiguousarray(sel.reshape(32, 4096))


_NC_CACHE = {}


def kernel(x, attn_norm_g, w_in, conv_w, conv_out_g, q_norm_g, k_norm_g,
           lambda_q1, lambda_k1, lambda_q2, lambda_k2, attn_subln_g, w_out,
           ffn_norm_g, w_router_group, w_router_expert, w_exp_gate, w_exp_up, w_exp_down):
    x = np.asarray(x, dtype=np.float32)
    f = lambda a: np.ascontiguousarray(np.asarray(a, dtype=np.float32))
    consts, sel = _host_consts()
    shared = {
        "consts": consts, "sel": sel,
        "attn_norm_g": f(attn_norm_g).reshape(1024), "w_in": f(w_in).reshape(1024, 3072),
        "conv_w": f(conv_w).reshape(3, 512), "conv_out_g": f(conv_out_g).reshape(512),
        "q_norm_g": f(q_norm_g).reshape(64), "k_norm_g": f(k_norm_g).reshape(64),
        "lambda_q1": f(lambda_q1).reshape(64), "lambda_k1": f(lambda_k1).reshape(64),
        "lambda_q2": f(lambda_q2).reshape(64), "lambda_k2": f(lambda_k2).reshape(64),
        "attn_subln_g": f(attn_subln_g).reshape(128), "w_out": f(w_out).reshape(1024, 1024),
        "ffn_norm_g": f(ffn_norm_g).reshape(1024),
        "w_router_group": f(w_router_group).reshape(1024, 4), "w_router_expert": f(w_router_expert).reshape(1024, 32),
        "w_exp_gate": f(w_exp_gate).reshape(32, 1024, 512), "w_exp_up": f(w_exp_up).reshape(32, 1024, 512),
        "w_exp_down": f(w_exp_down).reshape(32, 512, 1024),
    }
    in_maps = []
    for core in range(8):
        b, half = core // 2, core % 2
        xb = x[b].reshape(32, 128, 1024)
        order = []
        halo = np.zeros((16, 2, 1024), np.float32)
        for i in range(16):
            own = _own_block(i, half)
            other = 4 * i + 1 - own
            order += [own, other]
            if own > 0:
                halo[i] = xb[own - 1, 126:128, :]
        xk = np.ascontiguousarray(xb[order].reshape(4096, 1024))
        ob = np.zeros((128, 2), np.float32)
        for par in range(2):
            visible = ((par + half) % 2) == 1
            ob[:, par] = 0.0 if visible else -30000.0
        m = dict(shared)
        m.update({"xk": xk, "xh": np.ascontiguousarray(halo.reshape(32, 1024)), "obias": ob})
        in_maps.append(m)
    if "nc" not in _NC_CACHE:
        _NC_CACHE["nc"] = build_program()
    res = run_bass_kernel_spmd(_NC_CACHE["nc"], in_maps, core_ids=list(range(8)))
    outp = np.empty((4, 32, 128, 1024), np.float32)
    for core in range(8):
        b, half = core // 2, core % 2
        o = np.asarray(res.results[core]["out"]).reshape(16, 128, 1024)
        for i in range(16):
            outp[b, _own_block(i, half)] = o[i]
    return outp.reshape(4, 4096, 1024)
```
